# Optimizing a Trainium2 kernel written in Bass

```python
import jax, jax.numpy as jnp
from jax import lax
import numpy as np

D_MODEL = 1024
BATCH = 8
SEQ = 2048
DEPTH = 2

CHUNK = 64
Q_BLOCK = 128
EPS = 1e-6
GLA_HEADS = 4
GLA_DK = 48
GLA_DV = 96
GLA_RANK = 16
GLA_TAU = 16.0
SB_HEADS = 6
SB_DH = 64
CONV_CH = 256
CONV_WIDTH = 31
D_FF = 2816
N_MOD = 9

GLA_QK = GLA_HEADS * GLA_DK
GLA_V = GLA_HEADS * GLA_DV
SB_W = SB_HEADS * SB_DH
MIX_W = GLA_V + SB_W + CONV_CH
IN_COLS = 2 * GLA_QK + 2 * GLA_V + GLA_RANK + 3 * SB_W + 2 * CONV_CH

kernel_name = "hybrid_gla_stickbreak_conformer_block"


def rms_norm(x, g):
    xf = x.astype(jnp.float32)
    y = xf * lax.rsqrt(jnp.mean(xf * xf, axis=-1, keepdims=True) + EPS)
    return (y * g.astype(jnp.float32)).astype(x.dtype)


def layer_norm(x, g, b):
    xf = x.astype(jnp.float32)
    mu = jnp.mean(xf, axis=-1, keepdims=True)
    xc = xf - mu
    y = xc * lax.rsqrt(jnp.mean(xc * xc, axis=-1, keepdims=True) + EPS)
    return (y * g.astype(jnp.float32) + b.astype(jnp.float32)).astype(x.dtype)


def modulate(x, g, shift, scale):
    return rms_norm(x, g) * (1.0 + scale[:, None, :]) + shift[:, None, :]


def swiglu(h, w_in, w_out):
    a, b = jnp.split(h @ w_in, 2, axis=-1)
    return (jax.nn.silu(a) * b) @ w_out


def split_heads(t, n_heads):
    B, T, _ = t.shape
    return t.reshape(B, T, n_heads, -1).transpose(0, 2, 1, 3)


def merge_heads(t):
    B, H, T, d = t.shape
    return t.transpose(0, 2, 1, 3).reshape(B, T, H * d)


def gla_chunked(q, k, v, log_a):
    B, H, T, DK = q.shape
    DV = v.shape[-1]
    NC = T // CHUNK

    def to_chunks(t):
        return jnp.moveaxis(t.reshape(B, H, NC, CHUNK, t.shape[-1]), 2, 0)

    qc, kc, vc, gc = to_chunks(q), to_chunks(k), to_chunks(v), to_chunks(log_a)
    causal = jnp.tril(jnp.ones((CHUNK, CHUNK), dtype=bool))

    def step(S, inp):
        qi, ki, vi, gi = inp
        b = jnp.cumsum(gi, axis=2)
        o_inter = jnp.einsum('bhtk,bhkv->bhtv', qi * jnp.exp(b), S)
        diff = b[:, :, :, None, :] - b[:, :, None, :, :]
        decay = jnp.exp(jnp.where(causal[:, :, None], diff, -jnp.inf))
        scores = jnp.einsum('bhtk,bhsk,bhtsk->bhts', qi, ki, decay)
        o_intra = jnp.einsum('bhts,bhsv->bhtv', scores, vi)
        b_last = b[:, :, -1:, :]
        k_dec = ki * jnp.exp(b_last - b)
        S_new = jnp.exp(b_last[:, :, 0, :])[..., None] * S + jnp.einsum('bhsk,bhsv->bhkv', k_dec, vi)
        return S_new, o_inter + o_intra

    S0 = jnp.zeros((B, H, DK, DV), jnp.float32)
    _, o = lax.scan(step, S0, (qc, kc, vc, gc))
    return jnp.moveaxis(o, 0, 2).reshape(B, H, T, DV)


def stick_breaking(q, k, v):
    T = q.shape[2]
    scale = SB_DH ** -0.5
    outs = []
    for i in range(T // Q_BLOCK):
        q0 = i * Q_BLOCK
        kv_len = q0 + Q_BLOCK
        qb = q[:, :, q0:kv_len]
        kb = k[:, :, :kv_len]
        vb = v[:, :, :kv_len]
        z = jnp.einsum('bhqd,bhkd->bhqk', qb, kb) * scale
        t_idx = q0 + jnp.arange(Q_BLOCK)[:, None]
        s_idx = jnp.arange(kv_len)[None, :]
        mask = s_idx < t_idx
        log_keep = jnp.where(mask, jax.nn.log_sigmoid(-z), 0.0)
        cum = jnp.cumsum(log_keep, axis=-1)
        log_w = jax.nn.log_sigmoid(z) + (cum[..., -1:] - cum)
        w = jnp.where(mask, jnp.exp(log_w), 0.0)
        outs.append(jnp.einsum('bhqk,bhkd->bhqd', w, vb))
    return jnp.concatenate(outs, axis=2)


def causal_depthwise_conv(u, w, b):
    out = lax.conv_general_dilated(
        u, w[:, None, :], window_strides=(1,), padding=[(CONV_WIDTH - 1, 0)],
        dimension_numbers=('NWC', 'WIO', 'NWC'), feature_group_count=u.shape[-1])
    return out + b


def hybrid_mixer(h, w_in, w_out, gla_w_gate_up, gla_b_gate, gla_out_norm,
                 sb_q_norm, sb_k_norm, sb_out_norm, conv_w, conv_b, conv_ln_g, conv_ln_b):
    dt = h.dtype
    proj = h @ w_in
    idx = [GLA_QK, 2 * GLA_QK, 2 * GLA_QK + GLA_V, 2 * GLA_QK + 2 * GLA_V,
           2 * GLA_QK + 2 * GLA_V + GLA_RANK]
    idx = idx + [idx[-1] + SB_W, idx[-1] + 2 * SB_W, idx[-1] + 3 * SB_W]
    g_q, g_k, g_v, g_g, g_r, s_q, s_k, s_v, c_u = jnp.split(proj, idx, axis=-1)

    log_a = jax.nn.log_sigmoid(g_r @ gla_w_gate_up + gla_b_gate) / GLA_TAU
    qa = split_heads(g_q, GLA_HEADS).astype(jnp.float32) * (GLA_DK ** -0.5)
    ka = split_heads(g_k, GLA_HEADS).astype(jnp.float32)
    va = split_heads(g_v, GLA_HEADS).astype(jnp.float32)
    la = split_heads(log_a, GLA_HEADS).astype(jnp.float32)
    oa = rms_norm(gla_chunked(qa, ka, va, la), gla_out_norm)
    out_a = merge_heads(oa).astype(dt) * jax.nn.silu(g_g)

    qb = rms_norm(split_heads(s_q, SB_HEADS), sb_q_norm).astype(jnp.float32)
    kb = rms_norm(split_heads(s_k, SB_HEADS), sb_k_norm).astype(jnp.float32)
    vb = split_heads(s_v, SB_HEADS).astype(jnp.float32)
    ob = rms_norm(stick_breaking(qb, kb, vb), sb_out_norm)
    out_b = merge_heads(ob).astype(dt)

    cu_a, cu_g = jnp.split(c_u, 2, axis=-1)
    u = cu_a * jax.nn.sigmoid(cu_g)
    u = causal_depthwise_conv(u, conv_w, conv_b)
    out_c = jax.nn.silu(layer_norm(u, conv_ln_g, conv_ln_b))

    return jnp.concatenate([out_a, out_b, out_c], axis=-1) @ w_out


def setup_inputs(seed: int = 0) -> dict:
    key = jax.random.key(seed)
    ks = jax.random.split(key, 24)
    L, D = DEPTH, D_MODEL

    def nrm(k, shape, s):
        return jax.random.normal(k, shape, jnp.float32) * s

    def gain(k, shape):
        return 1.0 + 0.02 * jax.random.normal(k, shape, jnp.float32)

    return {
        "x": nrm(ks[0], (BATCH, SEQ, D), 1.0),
        "c": nrm(ks[1], (BATCH, D), 1.0),
        "w_ada": nrm(ks[2], (L, D, N_MOD * D), D ** -0.5),
        "b_ada": nrm(ks[3], (L, N_MOD * D), 0.02),
        "norm_ffn1": gain(ks[4], (L, D)),
        "ffn1_w_in": nrm(ks[5], (L, D, 2 * D_FF), D ** -0.5),
        "ffn1_w_out": nrm(ks[6], (L, D_FF, D), D_FF ** -0.5),
        "norm_mix": gain(ks[7], (L, D)),
        "w_in": nrm(ks[8], (L, D, IN_COLS), D ** -0.5),
        "w_out": nrm(ks[9], (L, MIX_W, D), MIX_W ** -0.5),
        "gla_w_gate_up": nrm(ks[10], (L, GLA_RANK, GLA_QK), GLA_RANK ** -0.5),
        "gla_b_gate": nrm(ks[11], (L, GLA_QK), 0.1),
        "gla_out_norm": gain(ks[12], (L, GLA_DV)),
        "sb_q_norm": gain(ks[13], (L, SB_DH)),
        "sb_k_norm": gain(ks[14], (L, SB_DH)),
        "sb_out_norm": gain(ks[15], (L, SB_DH)),
        "conv_w": nrm(ks[16], (L, CONV_WIDTH, CONV_CH), CONV_WIDTH ** -0.5),
        "conv_b": nrm(ks[17], (L, CONV_CH), 0.02),
        "conv_ln_g": gain(ks[18], (L, CONV_CH)),
        "conv_ln_b": nrm(ks[19], (L, CONV_CH), 0.02),
        "norm_ffn2": gain(ks[20], (L, D)),
        "ffn2_w_in": nrm(ks[21], (L, D, 2 * D_FF), D ** -0.5),
        "ffn2_w_out": nrm(ks[22], (L, D_FF, D), D_FF ** -0.5),
    }


def reference(x, c, w_ada, b_ada, norm_ffn1, ffn1_w_in, ffn1_w_out, norm_mix, w_in, w_out,
              gla_w_gate_up, gla_b_gate, gla_out_norm, sb_q_norm, sb_k_norm, sb_out_norm,
              conv_w, conv_b, conv_ln_g, conv_ln_b, norm_ffn2, ffn2_w_in, ffn2_w_out):
    c_act = jax.nn.silu(c)
    for l in range(DEPTH):
        mod = c_act @ w_ada[l] + b_ada[l]
        sh1, sc1, gt1, sh2, sc2, gt2, sh3, sc3, gt3 = jnp.split(mod, N_MOD, axis=-1)
        h = modulate(x, norm_ffn1[l], sh1, sc1)
        x = x + 0.5 * gt1[:, None, :] * swiglu(h, ffn1_w_in[l], ffn1_w_out[l])
        h = modulate(x, norm_mix[l], sh2, sc2)
        y = hybrid_mixer(h, w_in[l], w_out[l], gla_w_gate_up[l], gla_b_gate[l], gla_out_norm[l],
                         sb_q_norm[l], sb_k_norm[l], sb_out_norm[l],
                         conv_w[l], conv_b[l], conv_ln_g[l], conv_ln_b[l])
        x = x + gt2[:, None, :] * y
        h = modulate(x, norm_ffn2[l], sh3, sc3)
        x = x + 0.5 * gt3[:, None, :] * swiglu(h, ffn2_w_in[l], ffn2_w_out[l])
    return x
```

```python
import math
import os
import numpy as np
LVL = int(os.environ.get('LVL', '9'))
import concourse.bass as bass
import concourse.mybir as mybir
from concourse.bass_utils import run_bass_kernel_spmd

F32 = mybir.dt.float32
BF16 = mybir.dt.bfloat16
AF = mybir.ActivationFunctionType
ALU = mybir.AluOpType

SB_BASE = 16512
SB_END = 229376

D = 1024
T = 2048
DFF = 2816
NL = 2
EPS = 1e-6
NFF = 22
FF_GROUPS = [(0, 4), (4, 8), (8, 12), (12, 16), (16, 19), (19, 22)]
IN_COLS = 2832


class Res:
    __slots__ = ("name", "w", "r", "dsem", "dcnt")

    def __init__(self, name, init=None):
        self.name = name
        self.w = None
        self.r = dict(init) if init else {}
        self.dsem = None
        self.dcnt = 0

    def state(self):
        d = dict(self.r)
        if self.w is not None:
            s, v = self.w
            if d.get(s, 0) < v:
                d[s] = v
        return d


class Eng:
    def __init__(self, name, h, sem, is_pe=False):
        self.name = name
        self.h = h
        self.sem = sem
        self.n = 0
        self.seen = {}
        self.is_pe = is_pe
        self.nwaits = 0
        self.nins = 0


class Arena:
    def __init__(self, lo, hi, align):
        self.free = [(lo, hi)]
        self.align = align
        self.grave = []
        self.peak = 0
        self.lo = lo

    def alloc(self, size):
        size = (size + self.align - 1) // self.align * self.align
        for i, (a, b) in enumerate(self.free):
            if b - a >= size:
                self.free[i] = (a + size, b)
                if self.free[i][0] == self.free[i][1]:
                    del self.free[i]
                self.peak = max(self.peak, a + size - self.lo)
                deps = {}
                keep = []
                for (ga, gb, gd) in self.grave:
                    if ga < a + size and gb > a:
                        for s, v in gd.items():
                            if deps.get(s, 0) < v:
                                deps[s] = v
                    keep.append((ga, gb, gd))
                self.grave = keep
                return a, size, deps
        raise RuntimeError(f"arena OOM size={size} free={self.free}")

    def release(self, a, size, deps):
        self.grave.append((a, a + size, deps))
        self.free.append((a, a + size))
        self.free.sort()
        m = []
        for seg in self.free:
            if m and m[-1][1] == seg[0]:
                m[-1] = (m[-1][0], seg[1])
            else:
                m.append(seg)
        self.free = m


class Tile:
    def __init__(self, t, res, addr, size, arena):
        self.t = t
        self.res = res
        self.addr = addr
        self.size = size
        self.arena = arena

    def __getitem__(self, k):
        return self.t[k]

    @property
    def r(self):
        return self.res[0]


class FW:
    def __init__(self, nc):
        self.nc = nc
        self.pe = Eng("pe", nc.tensor, self.new_sem("s_pe"), is_pe=True)
        self.act = Eng("act", nc.scalar, self.new_sem("s_act"))
        self.dve = Eng("dve", nc.vector, self.new_sem("s_dve"))
        self.pool = Eng("pool", nc.gpsimd, self.new_sem("s_pool"))
        self.sp = Eng("sp", nc.sync, self.new_sem("s_sp"))
        self.engs = [self.pe, self.act, self.dve, self.pool, self.sp]
        self.sb = Arena(SB_BASE, SB_END, 64)
        self.ps = Arena(0, 4096, 512)
        self.pst = nc.alloc_psum_tensor("ps", [128, 4096], F32)
        self.uid = 0
        self.out_sems = []
        self.free_dsems = []

    def new_sem(self, name):
        return self.nc.alloc_semaphore(name)

    def tile(self, name, shape, dtype, nres=1):
        nbytes = int(np.prod(shape[1:])) * (2 if dtype == BF16 else 4)
        a, size, deps = self.sb.alloc(nbytes)
        self.uid += 1
        t = self.nc.alloc_sbuf_tensor_at(f"{name}_{self.uid}", list(shape), dtype, offset=a)
        res = [Res(f"{name}{i}", deps) for i in range(nres)]
        return Tile(t, res, a, size, self.sb)

    def ptile(self, name, ncols=512, nres=1):
        a, size, deps = self.ps.alloc(ncols)
        res = [Res(f"{name}{i}", deps) for i in range(nres)]
        return Tile(self.pst[:, a:a + size], res, a, size, self.ps)

    def free(self, *tls):
        for tl in tls:
            deps = {}
            for r in tl.res:
                for s, v in r.state().items():
                    if deps.get(s, 0) < v:
                        deps[s] = v
                if r.dsem is not None:
                    self.free_dsems.append((r.dsem, r.dcnt))
                    r.dsem = None
            tl.arena.release(tl.addr, tl.size, deps)

    def _waits(self, eng, reads, writes):
        need = {}

        def add(s, v):
            if need.get(s, 0) < v:
                need[s] = v

        for r in reads:
            if r.w is not None:
                add(*r.w)
        for w in writes:
            if w.w is not None:
                add(*w.w)
            for s, v in w.r.items():
                if s is eng.sem:
                    continue
                add(s, v)
        for s, v in need.items():
            if eng.is_pe and s is eng.sem:
                continue
            if eng.seen.get(s, 0) >= v:
                continue
            eng.h.wait_ge(s, v)
            eng.nwaits += 1
            eng.seen[s] = v

    def op(self, eng, fn, reads=(), writes=()):
        self._waits(eng, reads, writes)
        ins = fn(eng.h)
        eng.n += 1
        ins.then_inc(eng.sem, 1)
        for r in reads:
            if r.r.get(eng.sem, 0) < eng.n:
                r.r[eng.sem] = eng.n
        for w in writes:
            w.w = (eng.sem, eng.n)
            w.r = {}

    def _get_dsem(self, r):
        if r.dsem is None:
            if self.free_dsems:
                r.dsem, r.dcnt = self.free_dsems.pop()
            else:
                self.uid += 1
                r.dsem = self.new_sem(f"d_{self.uid}")
                r.dcnt = 0
        return r.dsem

    def dma_in(self, q, out_ap, in_ap, wres):
        self._waits(q, (), (wres,))
        sem = self._get_dsem(wres)
        q.h.dma_start(out=out_ap, in_=in_ap).then_inc(sem, 16)
        wres.dcnt += 16
        wres.w = (sem, wres.dcnt)
        wres.r = {}

    def dma_out(self, q, out_ap, in_ap, rres):
        self._waits(q, rres, ())
        self.uid += 1
        sem = self.new_sem(f"o_{self.uid}")
        q.h.dma_start(out=out_ap, in_=in_ap).then_inc(sem, 16)
        self.out_sems.append(sem)
        for r in rres:
            r.r[sem] = 16

    def finish(self):
        for s in self.out_sems:
            self.sp.h.wait_ge(s, 16)
        for e in self.engs:
            if e is self.sp or e.n == 0:
                continue
            self.sp.h.wait_ge(e.sem, e.n)


C_ONES, C_BD64, C_TRI_INCL, C_TRI_GT, C_TRI_SUFF, C_MSTRICT, C_IDENT, C_NTRI_SUFF, C_NONES = range(9)
NCONST = 9
SC_GLA_ON, SC_SBQ, SC_SBK, SC_SBO, SC_CB, SC_CLG, SC_CLB, SC_CW = 0, 1, 2, 3, 4, 6, 8, 10
SC_PER = 72


def make_consts():
    p = np.arange(128)
    c = np.zeros((128, NCONST, 128), np.float32)
    c[:, C_ONES] = 1.0
    c[:, C_BD64] = (p[:, None] // 64 == p[None, :] // 64)
    same = (p[:, None] // 64 == p[None, :] // 64)
    c[:, C_TRI_INCL] = same & (p[:, None] <= p[None, :])
    c[:, C_TRI_GT] = same & (p[:, None] > p[None, :])
    c[:, C_TRI_SUFF] = (p[:, None] >= p[None, :])
    c[:, C_MSTRICT] = (p[:, None] < p[None, :])
    c[:, C_IDENT] = np.eye(128)
    c[:, C_NTRI_SUFF] = -c[:, C_TRI_SUFF]
    c[:, C_NONES] = -1.0
    return c


class _Stop(Exception):
    pass


def build(layers=(0, 1), taps=(), stop=None):
    nc = bass.Bass("TRN2", target_bir_lowering=False)

    def din(name, shape):
        return nc.dram_tensor(name, list(shape), F32, kind="ExternalInput").ap()

    xT_d = din("xT", [D, T])
    ccol_d = din("ccol", [128, 8])
    wada_d = din("w_ada", [NL, D, 9 * D])
    bada_d = din("b_ada_col", [128, NL * 72])
    gains_d = din("gains", [128, NL * 24])
    f1wi_d = din("ffn1_w_in", [NL, D, 2 * DFF])
    f1wo_d = din("ffn1_w_out", [NL, DFF, D])
    f2wi_d = din("ffn2_w_in", [NL, D, 2 * DFF])
    f2wo_d = din("ffn2_w_out", [NL, DFF, D])
    win_d = din("w_in", [NL, D, IN_COLS])
    wqk_d = din("w_in_qk", [NL, D, 512])
    wout_d = din("w_out", [NL, D, D])
    gup_d = din("gla_up_pad", [NL, 16, 256])
    gbg_d = din("gla_bg_pad", [NL, 1, 256])
    sc_d = din("smallcols", [128, NL * SC_PER])
    consts_d = din("consts", [128, NCONST * 128])
    oT_d = nc.dram_tensor("oT", [D, T], F32, kind="ExternalOutput").ap()
    tap_d = {}

    fw = FW(nc)
    pe, act, dve, pool, sp = fw.pe, fw.act, fw.dve, fw.pool, fw.sp
    op = fw.op

    def memrep(label):
        if os.environ.get('DEBUGMEM'):
            used = (SB_END - SB_BASE) - sum(b - a for a, b in fw.sb.free)
            print(f'[mem] {label}: used {used/1024:.1f} KB peak {fw.sb.peak/1024:.1f}')

    def maybe_stop(label):
        memrep(label)
        if stop == label:
            raise _Stop()

    def tap(name, tl, shape, dtype=F32):
        if name not in taps:
            return
        d = nc.dram_tensor("tap_" + name, list(shape), dtype, kind="ExternalOutput").ap()
        tap_d[name] = d
        src = tl.t
        if len(shape) == 2:
            src = src[:, :]
        elif len(shape) == 3:
            src = src[:, :, :]
        fw.dma_out(sp, d, src, tl.res)

    CF = fw.tile("CF", [128, NCONST, 128], F32)
    CB = fw.tile("CB", [128, NCONST, 128], BF16)
    SC = fw.tile("SC", [128, NL * SC_PER], F32)
    GN = fw.tile("GN", [128, NL * 24], F32)
    BA = fw.tile("BA", [128, NL * 72], F32)
    CC = fw.tile("CC", [128, 8], F32)
    fw.dma_in(sp, CF.t.rearrange("p a b -> p (a b)"), consts_d, CF.r)
    fw.dma_in(pool, CB.t.rearrange("p a b -> p (a b)"), consts_d, CB.r)
    fw.dma_in(sp, SC[:, :], sc_d, SC.r)
    fw.dma_in(sp, GN[:, :], gains_d, GN.r)
    fw.dma_in(sp, BA[:, :], bada_d, BA.r)
    fw.dma_in(sp, CC[:, :], ccol_d, CC.r)

    def cb(i):
        return CB[:, i, :]

    def cf(i):
        return CF[:, i, :]

    X = fw.tile("X", [128, 8, T], F32, nres=8)
    for r8 in range(8):
        fw.dma_in(sp, X[:, :, r8 * 256:(r8 + 1) * 256],
                  xT_d[:, r8 * 256:(r8 + 1) * 256].rearrange("(k p) t -> p k t", p=128), X.res[r8])

    def xr(t0, t1):
        return [X.res[i] for i in range(t0 // 256, (t1 + 255) // 256)]

    class _Holder:
        tl = None

        def __getitem__(self, k):
            return self.tl.t[k]

        @property
        def t(self):
            return self.tl.t

        @property
        def res(self):
            return self.tl.res

    H = _Holder()

    def h_alloc():
        H.tl = fw.tile("H", [128, 8, T], BF16, nres=8)

    def h_free():
        fw.free(H.tl)
        H.tl = None

    def hr(t0, t1):
        return [H.res[i] for i in range(t0 // 256, (t1 + 255) // 256)]

    CA = fw.tile("CA", [128, 8], BF16)
    op(act, lambda a: a.activation(out=CA[:, :], in_=CC[:, :], func=AF.Silu), [CC.r], [CA.r])
    MOD = {}
    DER = {}
    for l in layers:
        MOD[l] = fw.tile("MOD", [128, 72], F32, nres=9)
        DER[l] = fw.tile("DER", [128, 6, 8], F32, nres=6)
    ada = {"WAb": [fw.tile("WAda", [128, 8, 1024], BF16) for _ in range(2)], "MP": fw.ptile("MP"),
           "list": [(l, ch) for l in layers for ch in range(9)], "nf": 0, "nc": 0}

    def ada_prefetch():
        i = ada["nf"]
        if i >= len(ada["list"]):
            return
        ada["nf"] += 1
        l, ch = ada["list"][i]
        WA = ada["WAb"][i % 2]
        fw.dma_in(pool, WA[:, :, :], wada_d[l, :, ch * 1024:(ch + 1) * 1024].rearrange("(k p) c -> p k c", p=128), WA.r)

    def ada_compute():
        i = ada["nc"]
        if i >= len(ada["list"]):
            return False
        ada["nc"] += 1
        l, ch = ada["list"][i]
        WA = ada["WAb"][i % 2]
        MP = ada["MP"]
        pc = (i % 8) * 8

        def f(pe_):
            last = None
            for jj in range(8):
                for k in range(8):
                    last = pe_.matmul(MP[:, pc + jj:pc + jj + 1], WA[:, k, jj * 128:(jj + 1) * 128],
                                      CA[:, k:k + 1], start=(k == 0), stop=(k == 7))
            return last
        op(pe, f, [WA.r, CA.r], [MP.r])
        op(dve, lambda v: v.tensor_tensor(out=MOD[l][:, ch * 8:(ch + 1) * 8], in0=MP[:, pc:pc + 8],
                                          in1=BA[:, l * 72 + ch * 8:l * 72 + (ch + 1) * 8], op=ALU.add),
           [MP.r, BA.r], [MOD[l].res[ch]])
        n = ch // 3
        if ch % 3 == 1:
            op(dve, lambda v: v.tensor_scalar(out=DER[l][:, n, :], in0=MOD[l][:, ch * 8:(ch + 1) * 8],
                                              scalar1=1.0, scalar2=float(math.sqrt(D)), op0=ALU.add, op1=ALU.mult),
               [MOD[l].res[ch]], [DER[l].res[n]])
            op(dve, lambda v: v.tensor_tensor(out=DER[l][:, n, :], in0=DER[l][:, n, :],
                                              in1=GN[:, (l * 3 + n) * 8:(l * 3 + n + 1) * 8], op=ALU.mult),
               [DER[l].res[n], GN.r], [DER[l].res[n]])
        if ch % 3 == 2:
            op(dve, lambda v: v.tensor_scalar(out=DER[l][:, 3 + n, :], in0=MOD[l][:, ch * 8:(ch + 1) * 8],
                                              scalar1=(1.0 if n == 1 else 0.5), scalar2=None, op0=ALU.mult),
               [MOD[l].res[ch]], [DER[l].res[3 + n]])
        if ada["nc"] == len(ada["list"]):
            fw.free(ada["MP"], *ada["WAb"])
        return True

    def ada_step():
        if ada_compute():
            ada_prefetch()

    ada_prefetch()
    ada_prefetch()
    for _ in range(3):
        ada_step()

    def rstd_from_psum(SS, np_, nelem, out_tile, w=512):
        LNT = fw.tile("LNT", [128, w], F32)
        op(act, lambda a: a.activation(out=LNT[0:np_, 0:w], in_=SS[0:np_, 0:w], func=AF.Ln, bias=float(nelem * EPS)),
           [SS.r], [LNT.r])
        op(act, lambda a: a.activation(out=out_tile[0:np_, 0:w], in_=LNT[0:np_, 0:w], func=AF.Exp, scale=-0.5),
           [LNT.r], [out_tile.r])
        fw.free(LNT)

    def norm_begin(l, n, nss=2):
        if H.tl is None:
            h_alloc()
        return dict(l=l, n=n, cnt=0,
                    SQs=[fw.tile("SQ", [128, 512], BF16) for _ in range(4)],
                    TMs=[fw.tile("TM", [128, 512], F32) for _ in range(3)],
                    RSs=[fw.tile("RS", [128, 512], F32) for _ in range(2)],
                    SSs=[fw.ptile("SS") for _ in range(nss)])

    def norm_chunk(ctx, c4):
        l, n = ctx["l"], ctx["n"]
        A = DER[l][:, n, :]
        B = MOD[l][:, (3 * n) * 8:(3 * n + 1) * 8]
        t0, t1 = c4 * 512, (c4 + 1) * 512
        SS = ctx["SSs"][c4 % len(ctx["SSs"])]
        RS = ctx["RSs"][c4 % 2]
        for k in range(8):
            SQ = ctx["SQs"][ctx["cnt"] % 4]
            ctx["cnt"] += 1
            if k % 2 == 0:
                op(act, lambda a, SQ=SQ, k=k: a.activation(out=SQ[:, :], in_=X[:, k, t0:t1], func=AF.Square),
                   xr(t0, t1), [SQ.r])
            else:
                op(dve, lambda v, SQ=SQ, k=k: v.tensor_tensor(out=SQ[:, :], in0=X[:, k, t0:t1], in1=X[:, k, t0:t1], op=ALU.mult),
                   xr(t0, t1), [SQ.r])
            op(pe, lambda p, SQ=SQ, k=k, SS=SS: p.matmul(SS[:, :], cb(C_ONES), SQ[:, :], start=(k == 0), stop=(k == 7)),
               [SQ.r, CB.r], [SS.r])
        rstd_from_psum(SS, 128, D, RS)
        for k in range(8):
            TM = ctx["TMs"][ctx["cnt"] % 3]
            ctx["cnt"] += 1
            op(dve, lambda v, TM=TM, k=k, RS=RS: v.tensor_tensor(out=TM[:, :], in0=X[:, k, t0:t1], in1=RS[:, :], op=ALU.mult),
               xr(t0, t1) + [RS.r], [TM.r])
            op(act, lambda a, TM=TM, k=k: a.activation(out=H[:, k, t0:t1], in_=TM[:, :], func=AF.Identity,
                                                       scale=A[:, k:k + 1], bias=B[:, k:k + 1]),
               [TM.r, DER[l].res[n], MOD[l].res[3 * n]], hr(t0, t1))

    def norm_end(ctx):
        fw.free(*ctx["SQs"], *ctx["TMs"], *ctx["RSs"], *ctx["SSs"])

    def norm_modulate(l, n):
        ctx = norm_begin(l, n)
        for c4 in range(4):
            norm_chunk(ctx, c4)
        norm_end(ctx)

    def ffn(l, which, pre_normed=False, next_norm=None):
        wi_d = f1wi_d if which == 0 else f2wi_d
        wo_d = f1wo_d if which == 0 else f2wo_d
        n = 0 if which == 0 else 2
        G = DER[l][:, 3 + n, :]
        if not pre_normed:
            norm_modulate(l, n)
        nctx = [None]
        tap(f"h{l}_{which}", H, [128, 8, T], BF16)
        WA = [fw.tile("FWa", [128, 8, 512], BF16) for _ in range(2)]
        WB = [fw.tile("FWb", [128, 8, 512], BF16) for _ in range(2)]
        WO = [fw.tile("FWo", [128, 4, 1024], BF16) for _ in range(2)]
        SAs = [fw.tile("SA", [128, 256], F32) for _ in range(3)]
        GTs = [fw.tile("GT", [128, 256], BF16) for _ in range(3)]
        ABs = [fw.ptile("AB") for _ in range(3)]
        Y = fw.ptile("Y", 2048)

        def load(gi):
            c0, c1 = FF_GROUPS[gi]
            nchk = c1 - c0
            s = gi % 2
            fw.dma_in(pool, WA[s][:, :, 0:nchk * 128],
                      wi_d[l, :, c0 * 128:c1 * 128].rearrange("(k p) c -> p k c", p=128), WA[s].r)
            fw.dma_in(pool, WB[s][:, :, 0:nchk * 128],
                      wi_d[l, :, DFF + c0 * 128:DFF + c1 * 128].rearrange("(k p) c -> p k c", p=128), WB[s].r)
            fw.dma_in(pool, WO[s][:, 0:nchk, :],
                      wo_d[l, c0 * 128:c1 * 128, :].rearrange("(f p) c -> p f c", p=128), WO[s].r)

        load(0)
        cnt = 0
        it_count = [0]
        for gi, (c0, c1) in enumerate(FF_GROUPS):
            s = gi % 2
            if gi + 1 < len(FF_GROUPS):
                load(gi + 1)
            nchk = c1 - c0
            def emit_ab(tt, fi, cidx):
                t0, t1 = tt * 256, (tt + 1) * 256
                AB = ABs[cidx % 3]

                def f(p):
                    last = None
                    for k in range(8):
                        p.matmul(AB[:, 0:256], WA[s][:, k, fi * 128:(fi + 1) * 128], H[:, k, t0:t1],
                                 start=(k == 0), stop=(k == 7))
                    for k in range(8):
                        last = p.matmul(AB[:, 256:512], WB[s][:, k, fi * 128:(fi + 1) * 128], H[:, k, t0:t1],
                                        start=(k == 0), stop=(k == 7))
                    return last
                op(pe, f, [WA[s].r, WB[s].r] + hr(t0, t1), [AB.r])
                return AB

            def emit_rest(tt, fi, cidx, AB):
                t0, t1 = tt * 256, (tt + 1) * 256
                SA = SAs[cidx % 3]
                GT = GTs[cidx % 3]
                op(act, lambda a: a.activation(out=SA[:, :], in_=AB[:, 0:256], func=AF.Silu), [AB.r], [SA.r])
                op(dve, lambda v: v.tensor_tensor(out=GT[:, :], in0=AB[:, 256:512], in1=SA[:, :], op=ALU.mult),
                   [AB.r, SA.r], [GT.r])

                def f(p):
                    last = None
                    for d in range(8):
                        last = p.matmul(Y[:, d * 256:(d + 1) * 256], WO[s][:, fi, d * 128:(d + 1) * 128], GT[:, :],
                                        start=(fi == 0 and d % 2 == 0), stop=(fi == nchk - 1), skip_group_check=True)
                    return last
                op(pe, f, [WO[s].r, GT.r], [Y.r])
                if fi == nchk - 1:
                    for d in range(8):
                        op(dve, lambda v, d=d: v.scalar_tensor_tensor(out=X[:, d, t0:t1], in0=Y[:, d * 256:(d + 1) * 256],
                                                                       scalar=G[:, d:d + 1], in1=X[:, d, t0:t1],
                                                                       op0=ALU.mult, op1=ALU.add),
                           [Y.r, DER[l].res[3 + n]] + xr(t0, t1), xr(t0, t1))
                    it_count[0] += 1
                    if it_count[0] % 2 == 0:
                        ada_step()
                    if next_norm is not None and gi == len(FF_GROUPS) - 1:
                        if nctx[0] is None:
                            nctx[0] = norm_begin(next_norm[0], next_norm[1], nss=1)
                        if tt % 2 == 1:
                            norm_chunk(nctx[0], tt // 2)

            seq = [(tt, fi) for tt in range(8) for fi in range(nchk)]
            prev = None
            for (tt, fi) in seq:
                AB = emit_ab(tt, fi, cnt)
                if prev is not None:
                    emit_rest(*prev)
                prev = (tt, fi, cnt, AB)
                cnt += 1
            emit_rest(*prev)
        fw.free(*WA, *WB, *WO, *SAs, *GTs, *ABs, Y)
        if nctx[0] is not None:
            norm_end(nctx[0])
        else:
            h_free()

    def proj_fm(Wt, col0, ncol, t0, t1, P, prow=None):
        def f(p):
            last = None
            for k in range(8):
                last = p.matmul(P[0:ncol, 0:t1 - t0], Wt[:, k, col0:col0 + ncol], H[:, k, t0:t1],
                                start=(k == 0), stop=(k == 7))
            return last
        op(pe, f, [Wt.r] + hr(t0, t1), [P.r])

    def proj_tm(Wt, col0, ncol, tok0, P, pcol0=0):
        def f(p):
            last = None
            for k in range(8):
                last = p.matmul(P[:, pcol0:pcol0 + ncol], H[:, k, tok0:tok0 + 128], Wt[:, k, col0:col0 + ncol],
                                start=(k == 0), stop=(k == 7))
            return last
        op(pe, f, [Wt.r] + hr(tok0, tok0 + 128), [P.r])

    def gla(l):
        sc0 = l * SC_PER
        WR = fw.tile("WR", [128, 8, 16], BF16)
        WUP = fw.tile("WUP", [16, 256], BF16)
        BG = fw.tile("BG", [1, 256], BF16)
        fw.dma_in(pool, WR[:, :, :], win_d[l, :, 1152:1168].rearrange("(k p) c -> p k c", p=128), WR.r)
        fw.dma_in(pool, WUP[:, :], gup_d[l], WUP.r)
        fw.dma_in(pool, BG[:, :], gbg_d[l], BG.r)
        WQK = fw.tile("WQK", [128, 8, 512], BF16)
        fw.dma_in(pool, WQK[:, :, :], wqk_d[l].rearrange("(k p) c -> p k c", p=128), WQK.r)
        GRT = fw.tile("GRT", [16, T], BF16)
        LA = fw.tile("LA", [128, 16, 256], BF16)
        Pa = [fw.ptile("Pa") for _ in range(2)]
        for c4 in range(4):
            P = Pa[c4 % 2]
            proj_fm(WR, 0, 16, c4 * 512, (c4 + 1) * 512, P)
            op(dve, lambda v, P=P, c4=c4: v.tensor_copy(out=GRT[0:16, c4 * 512:(c4 + 1) * 512], in_=P[0:16, :]), [P.r], [GRT.r])
        EZ = [fw.tile("EZ", [128, 512], F32) for _ in range(2)]
        for t2 in range(8):
            P = Pa[t2 % 2]

            def f(p, P=P, t2=t2):
                last = None
                for i in range(2):
                    tt = 2 * t2 + i
                    p.matmul(P[:, i * 256:(i + 1) * 256], GRT[0:16, tt * 128:(tt + 1) * 128], WUP[0:16, :], start=True, stop=False)
                    last = p.matmul(P[:, i * 256:(i + 1) * 256], CB[0:1, C_ONES, :], BG[0:1, :], start=False, stop=True)
                return last
            op(pe, f, [GRT.r, WUP.r, BG.r, CB.r], [P.r])
            E = EZ[t2 % 2]
            op(act, lambda a, P=P, E=E: a.activation(out=E[:, :], in_=P[:, :], func=AF.Exp, scale=-1.0), [P.r], [E.r])
            op(act, lambda a, E=E, t2=t2: a.activation(out=LA[:, 2 * t2:2 * t2 + 2, :].rearrange("p a b -> p (a b)"), in_=E[:, :],
                                                       func=AF.Ln, bias=1.0), [E.r], [LA.r])
        fw.free(WR, WUP, BG, GRT, *EZ)
        tap(f"la{l}", LA, [128, 16, 256], BF16)
        maybe_stop(f"glaA_{l}")
        QT = fw.tile("GQT", [128, 2, T], BF16)
        KT = fw.tile("GKT", [128, 2, T], BF16)
        DL = fw.tile("DL", [128, 2, 32], F32)
        KDL = fw.tile("KDL", [128, 16, 256], BF16)
        KDH = fw.tile("KDH", [128, 16, 256], BF16)
        Pb = [fw.ptile("Pb") for _ in range(2)]
        Pq = [fw.ptile("Pq") for _ in range(2)]
        EB = [fw.tile("EB", [128, 512], F32) for _ in range(2)]
        cnt = 0
        lnscale = float(math.log(48 ** -0.5))
        for c4 in range(4):
            t0, t1 = c4 * 512, (c4 + 1) * 512
            for j in range(2):
                PB = Pb[cnt % 2]

                def f(p, PB=PB, j=j, c4=c4):
                    last = None
                    for i in range(4):
                        tt = 4 * c4 + i
                        last = p.matmul(PB[:, i * 128:(i + 1) * 128], LA[:, tt, 128 * j:128 * j + 128], cb(C_TRI_INCL),
                                        start=True, stop=True)
                    return last
                op(pe, f, [LA.r, CB.r], [PB.r])
                op(act, lambda a, PB=PB, j=j, c4=c4: a.activation(out=DL[:, j, 8 * c4:8 * c4 + 8], in_=PB[:, 63::64],
                                                                   func=AF.Exp, scale=-1.0 / 16), [PB.r], [DL.r])
                for qk in range(2):
                    E = EB[cnt % 2]
                    PQ = Pq[cnt % 2]
                    cnt += 1
                    if qk == 0:
                        op(act, lambda a, PB=PB, E=E: a.activation(out=E[:, :], in_=PB[:, :], func=AF.Exp, scale=-1.0 / 16,
                                                                   bias=lnscale), [PB.r], [E.r])
                    else:
                        op(act, lambda a, PB=PB, E=E: a.activation(out=E[:, :], in_=PB[:, :], func=AF.Exp, scale=1.0 / 16),
                           [PB.r], [E.r])
                    proj_fm(WQK, qk * 256 + j * 128, 128, t0, t1, PQ)
                    dst = QT if qk == 0 else KT
                    op(dve, lambda v, PQ=PQ, E=E, dst=dst, j=j: v.tensor_tensor(out=dst[:, j, t0:t1], in0=PQ[:, :], in1=E[:, :],
                                                                                 op=ALU.mult), [PQ.r, E.r], [dst.r])
        for t2 in range(8):
            PK = Pq[t2 % 2]
            PB = Pb[t2 % 2]
            for i in range(2):
                proj_tm(WQK, 256, 256, (2 * t2 + i) * 128, PK, pcol0=i * 256)

            def f(p, PB=PB, t2=t2):
                last = None
                for i in range(2):
                    last = p.matmul(PB[:, i * 256:(i + 1) * 256], cb(C_TRI_GT), LA[:, 2 * t2 + i, :], start=True, stop=True)
                return last
            op(pe, f, [LA.r, CB.r], [PB.r])
            E = EB[t2 % 2]
            op(act, lambda a, PB=PB, E=E: a.activation(out=E[:, :], in_=PB[:, :], func=AF.Exp, scale=-1.0 / 16), [PB.r], [E.r])
            for KDx, mc in ((KDL, 0), (KDH, 127)):
                op(dve, lambda v, PK=PK, E=E, t2=t2, KDx=KDx, mc=mc: v.scalar_tensor_tensor(
                    out=KDx[:, 2 * t2:2 * t2 + 2, :].rearrange("p a b -> p (a b)"), in0=PK[:, :],
                    scalar=CF[:, C_BD64, mc:mc + 1], in1=E[:, :], op0=ALU.mult, op1=ALU.mult), [PK.r, E.r, CF.r], [KDx.r])
        maybe_stop(f"glaB_{l}")
        fw.free(WQK, LA, *EB, *Pb)
        WV = fw.tile("WV", [128, 8, 384], BF16)
        fw.dma_in(pool, WV[:, :, :], win_d[l, :, 384:768].rearrange("(k p) c -> p k c", p=128), WV.r)
        WG = fw.tile("WG", [128, 8, 384], BF16)
        fw.dma_in(pool, WG[:, :, :], win_d[l, :, 768:1152].rearrange("(k p) c -> p k c", p=128), WG.r)
        VG = fw.tile("VG", [128, 16, 384], BF16)
        for tt in range(16):
            P = Pq[tt % 2]
            proj_tm(WV, 0, 384, tt * 128, P)
            op(act, lambda a, P=P, tt=tt: a.activation(out=VG[:, tt, :], in_=P[:, 0:384], func=AF.Identity), [P.r], [VG.r])
        SG = fw.tile("SG", [96, 4, T], BF16)
        cnt = 0
        for h in range(4):
            for c4 in range(4):
                P = Pq[cnt % 2]
                cnt += 1
                proj_fm(WG, 96 * h, 96, c4 * 512, (c4 + 1) * 512, P)
                op(act, lambda a, P=P, h=h, c4=c4: a.activation(out=SG[0:96, h, c4 * 512:(c4 + 1) * 512], in_=P[0:96, :], func=AF.Silu),
                   [P.r], [SG.r])
        fw.free(WV, WG, *Pq, *Pa)
        tap(f"gqt{l}", QT, [128, 2, T], BF16)
        tap(f"gkt{l}", KT, [128, 2, T], BF16)
        tap(f"dl{l}", DL, [128, 2, 32])
        maybe_stop(f"glaC_{l}")
        GC = fw.tile("GC", [128, 1], F32)
        op(dve, lambda v: v.tensor_scalar(out=GC[0:96, :], in0=SC[0:96, sc0 + SC_GLA_ON:sc0 + SC_GLA_ON + 1],
                                          scalar1=float(math.sqrt(96)), scalar2=None, op0=ALU.mult), [SC.r], [GC.r])
        OA = fw.tile("OA", [96, 4, T], BF16)
        S = [fw.tile("S", [128, 192], F32) for _ in range(2)]
        SBF = [[fw.tile("SBF", [128, 192], BF16) for _ in range(4)] for _ in range(2)]
        for j in range(2):
            op(dve, lambda v, j=j: v.memset(S[j][:, :], 0.0), [], [S[j].r])
        PU = fw.ptile("PU")
        PSs = [fw.ptile("PS") for _ in range(2)]
        PO = [fw.ptile("PO") for _ in range(4)]
        SSo = fw.ptile("SSo")
        PTs = [fw.tile("PT", [128, 128], BF16) for _ in range(4)]
        SQo = fw.tile("SQo", [128, 512], BF16)
        RSo = fw.tile("RSo", [128, 512], F32)
        T1 = fw.tile("T1", [128, 512], F32)
        cnt = 0
        for c4 in range(4):
            for i4 in range(4):
                tt = 4 * c4 + i4
                tk0 = tt * 128
                for j in range(2):
                    def fu(p, tt=tt, j=j):
                        last = None
                        for ci in range(2):
                            KDx = KDL if ci == 0 else KDH
                            last = p.matmul(PU[:, ci * 192:(ci + 1) * 192], KDx[:, tt, 128 * j:128 * j + 128],
                                            VG[:, tt, 192 * j:192 * j + 192], start=True, stop=True)
                        return last
                    op(pe, fu, [KDL.r, KDH.r, VG.r], [PU.r])
                    pts = []
                    for hh in range(2):
                        b0 = 64 * hh
                        PS_ = PSs[hh]
                        PT = PTs[cnt % 4]
                        cnt += 1
                        op(pe, lambda p, PS_=PS_, b0=b0, j=j: p.matmul(PS_[:, 0:128], KT[b0:b0 + 48, j, tk0:tk0 + 128],
                                                                        QT[b0:b0 + 48, j, tk0:tk0 + 128], start=True, stop=True),
                           [KT.r, QT.r], [PS_.r])
                        op(dve, lambda v, PS_=PS_, PT=PT: v.tensor_tensor(out=PT[:, :], in0=PS_[:, 0:128], in1=cf(C_TRI_INCL), op=ALU.mult),
                           [PS_.r, CF.r], [PT.r])
                        pts.append(PT)
                    sbf = []
                    for ci in range(2):
                        c = 2 * tt + ci
                        sb_ = SBF[j][c % 4]
                        sbf.append(sb_)
                        op(dve, lambda v, sb_=sb_, j=j: v.tensor_copy(out=sb_[:, :], in_=S[j][:, :]), [S[j].r], [sb_.r])
                        op(dve, lambda v, j=j, c=c, ci=ci: v.scalar_tensor_tensor(out=S[j][:, :], in0=S[j][:, :], scalar=DL[:, j, c:c + 1],
                                                                                   in1=PU[:, ci * 192:(ci + 1) * 192],
                                                                                   op0=ALU.mult, op1=ALU.add),
                           [S[j].r, DL.r, PU.r], [S[j].r])
                    for hh in range(2):
                        h = 2 * j + hh
                        b0 = 64 * hh
                        PT = pts[hh]

                        def fo(p, PT=PT, h=h, hh=hh, b0=b0, j=j, i4=i4, sbf=sbf, tt=tt):
                            o = PO[h]
                            p.matmul(o[0:96, i4 * 128:(i4 + 1) * 128], VG[:, tt, 96 * h:96 * h + 96], PT[:, :], start=True, stop=False)
                            last = None
                            for ci in range(2):
                                last = p.matmul(o[0:96, i4 * 128 + 64 * ci:i4 * 128 + 64 * ci + 64],
                                                sbf[ci][b0:b0 + 48, 96 * hh:96 * hh + 96],
                                                QT[b0:b0 + 48, j, tk0 + 64 * ci:tk0 + 64 * ci + 64], start=False, stop=True)
                            return last
                        op(pe, fo, [VG.r, PT.r, sbf[0].r, sbf[1].r, QT.r], [PO[h].r])
            t0, t1 = c4 * 512, (c4 + 1) * 512
            for h in range(4):
                if LVL < 4:
                    break
                op(act, lambda a, h=h: a.activation(out=SQo[0:96, :], in_=PO[h][0:96, :], func=AF.Square), [PO[h].r], [SQo.r])
                op(pe, lambda p: p.matmul(SSo[0:96, :], CB[0:96, C_ONES, 0:96], SQo[0:96, :], start=True, stop=True), [SQo.r, CB.r], [SSo.r])
                rstd_from_psum(SSo, 96, 96, RSo)
                op(dve, lambda v, h=h: v.tensor_tensor(out=T1[0:96, :], in0=PO[h][0:96, :], in1=RSo[0:96, :], op=ALU.mult),
                   [PO[h].r, RSo.r], [T1.r])
                op(dve, lambda v, h=h: v.scalar_tensor_tensor(out=OA[0:96, h, t0:t1], in0=T1[0:96, :], scalar=GC[0:96, 0:1],
                                                               in1=SG[0:96, h, t0:t1], op0=ALU.mult, op1=ALU.mult),
                   [T1.r, GC.r, SG.r], [OA.r])
        fw.free(QT, KT, DL, KDL, KDH, VG, SG, GC, *S, *SBF[0], *SBF[1], PU, *PSs, *PO, SSo, *PTs, SQo, RSo, T1)
        tap(f"oa{l}", OA, [96, 4, T], BF16)
        maybe_stop(f"glaD_{l}")
        return OA

    def sba(l):
        sc0 = l * SC_PER
        Wqk = [None, None, None]

        def wload(i):
            Wt = fw.tile("WSB", [128, 8, 384], BF16)
            fw.dma_in(pool, Wt[:, :, :], win_d[l, :, 1168 + 384 * i:1168 + 384 * (i + 1)].rearrange("(k p) c -> p k c", p=128), Wt.r)
            Wqk[i] = Wt
        wload(0)
        wload(1)
        QT = fw.tile("SQT", [128, 3, T], BF16)
        QTH = fw.tile("SQTH", [128, 3, T], BF16)
        KT = fw.tile("SKT", [128, 3, T], BF16)
        VS = fw.tile("VS", [128, 16, 384], BF16)
        GK = fw.tile("GK", [128, 3], F32)
        op(dve, lambda v: v.tensor_tensor(out=GK[:, 2:3], in0=SC[:, sc0 + SC_SBQ:sc0 + SC_SBQ + 1], in1=CF[:, C_BD64, 127:128], op=ALU.mult),
           [SC.r, CF.r], [GK.r])
        op(dve, lambda v: v.tensor_scalar(out=GK[:, 0:1], in0=SC[:, sc0 + SC_SBK:sc0 + SC_SBK + 1], scalar1=8.0, scalar2=None, op0=ALU.mult),
           [SC.r], [GK.r])
        op(dve, lambda v: v.tensor_scalar(out=GK[:, 1:2], in0=SC[:, sc0 + SC_SBO:sc0 + SC_SBO + 1], scalar1=8.0, scalar2=None, op0=ALU.mult),
           [SC.r], [GK.r])
        Pp = [fw.ptile("Pp") for _ in range(2)]
        SSp = [fw.ptile("SSp") for _ in range(2)]
        SQp = [fw.tile("SQp", [128, 512], BF16) for _ in range(2)]
        RSp = [fw.tile("RSp", [128, 512], F32) for _ in range(2)]
        cnt = 0
        for qk in range(2):
            dst = QT if qk == 0 else KT
            gcol = SC[:, sc0 + SC_SBQ:sc0 + SC_SBQ + 1] if qk == 0 else GK[:, 0:1]
            for j in range(3):
                for c4 in range(4):
                    t0, t1 = c4 * 512, (c4 + 1) * 512
                    P = Pp[cnt % 2]
                    SS = SSp[cnt % 2]
                    SQ = SQp[cnt % 2]
                    RS = RSp[cnt % 2]
                    cnt += 1
                    proj_fm(Wqk[qk], j * 128, 128, t0, t1, P)
                    op(act, lambda a, P=P, SQ=SQ: a.activation(out=SQ[:, :], in_=P[:, :], func=AF.Square), [P.r], [SQ.r])
                    op(pe, lambda p, SS=SS, SQ=SQ: p.matmul(SS[:, :], cb(C_BD64), SQ[:, :], start=True, stop=True), [SQ.r, CB.r], [SS.r])
                    rstd_from_psum(SS, 128, 64, RS)
                    op(dve, lambda v, P=P, RS=RS, dst=dst, j=j, gcol=gcol, t0=t0, t1=t1: v.scalar_tensor_tensor(
                        out=dst[:, j, t0:t1], in0=P[:, :], scalar=gcol, in1=RS[:, :], op0=ALU.mult, op1=ALU.mult),
                       [P.r, RS.r, SC.r, GK.r], [dst.r])
                    if qk == 0:
                        op(dve, lambda v, P=P, RS=RS, j=j, t0=t0, t1=t1: v.scalar_tensor_tensor(
                            out=QTH[:, j, t0:t1], in0=P[:, :], scalar=GK[:, 2:3], in1=RS[:, :], op0=ALU.mult, op1=ALU.mult),
                           [P.r, RS.r, GK.r], [QTH.r])
        fw.free(Wqk[0], Wqk[1])
        wload(2)
        for tt in range(16):
            P = Pp[tt % 2]
            proj_tm(Wqk[2], 0, 384, tt * 128, P)
            op(act, lambda a, P=P, tt=tt: a.activation(out=VS[:, tt, :], in_=P[:, 0:384], func=AF.Identity), [P.r], [VS.r])
        fw.free(Wqk[2], *Pp, *SSp, *SQp, *RSp)
        h_free()
        tap(f"sqt{l}", QT, [128, 3, T], BF16)
        tap(f"skt{l}", KT, [128, 3, T], BF16)
        tap(f"vs{l}", VS, [128, 16, 384], BF16)
        maybe_stop(f"sbaA_{l}")
        OB = fw.tile("OB", [128, 3, T], BF16)
        QC = 1024

        def segs(c0, c1):
            out = []
            c = c0
            while c < c1:
                e = min(c1, (c // 512 + 1) * 512)
                out.append((c, e))
                c = e
            return out
        Zp = [fw.ptile("Zp", QC) for _ in range(3)]
        PO_ = fw.ptile("POs", QC)
        Es = [fw.tile("E", [128, QC], F32) for _ in range(2)]
        SPs = [fw.tile("SP", [128, QC], BF16) for _ in range(4)]
        WTs = [fw.tile("WT", [128, QC], BF16) for _ in range(3)]
        LSB = [fw.tile("LSB", [128, QC], BF16) for _ in range(4)]
        OS = fw.tile("OS", [128, QC], F32)
        SQn = fw.tile("SQn", [128, QC], BF16)
        RSn = fw.tile("RSn", [128, QC], F32)
        its = []
        for qc in range(T // QC):
            q0 = qc * QC
            for j in range(3):
                for hh in range(2):
                    kbs = list(range((q0 + QC) // 128 - 1, -1, -1))
                    for ii, kb in enumerate(kbs):
                        its.append(dict(q0=q0, j=j, hh=hh, ii=ii, nk=len(kbs), kb=kb, col0=max(0, kb * 128 - q0)))
        NI = len(its)

        def stage1(g):
            it = its[g]
            q0, j, hh, ii, nk, kb, col0 = it["q0"], it["j"], it["hh"], it["ii"], it["nk"], it["kb"], it["col0"]
            Z = Zp[g % 3]
            E = Es[g % 2]
            SPt = SPs[g % 4]

            def fz(p):
                last = None
                for (a_, b_) in segs(col0, QC):
                    if hh == 0:
                        last = p.matmul(Z[:, a_:b_], KT[0:64, j, kb * 128:(kb + 1) * 128], QT[0:64, j, q0 + a_:q0 + b_],
                                        start=True, stop=False, skip_group_check=True)
                    else:
                        last = p.matmul(Z[:, a_:b_], KT[:, j, kb * 128:(kb + 1) * 128], QTH[:, j, q0 + a_:q0 + b_],
                                        start=True, stop=False, skip_group_check=True)
                return last
            op(pe, fz, [KT.r, QT.r, QTH.r], [Z.r])
            op(act, lambda a: a.activation(out=E[:, col0:QC], in_=Z[:, col0:QC], func=AF.Exp), [Z.r], [E.r])
            op(act, lambda a: a.activation(out=SPt[:, col0:QC], in_=E[:, col0:QC], func=AF.Ln, bias=1.0), [E.r], [SPt.r])
            if kb * 128 >= q0:
                op(dve, lambda v: v.tensor_tensor(out=SPt[:, col0:col0 + 128], in0=SPt[:, col0:col0 + 128],
                                                  in1=cf(C_MSTRICT), op=ALU.mult), [SPt.r, CF.r], [SPt.r])
            carry = None
            if ii + 1 < nk:
                if ii == 0:
                    carry = SPt
                else:
                    prev = its[g - 1]["carry"]
                    pc0 = its[g - 1]["col0"]
                    carry = LSB[g % 4]
                    op(dve, lambda v: v.tensor_tensor(out=carry[:, pc0:QC], in0=prev[:, pc0:QC], in1=SPt[:, pc0:QC], op=ALU.add),
                       [prev.r, SPt.r], [carry.r])
                    if col0 < pc0:
                        op(dve, lambda v: v.tensor_copy(out=carry[:, col0:pc0], in_=SPt[:, col0:pc0]), [SPt.r], [carry.r])
            it["Z"], it["SP"], it["carry"] = Z, SPt, carry

        def stage2(g):
            it = its[g]
            q0, kb, col0, Z, SPt = it["q0"], it["kb"], it["col0"], it["Z"], it["SP"]
            carry_in = its[g - 1]["carry"] if it["ii"] > 0 else None
            pc0 = its[g - 1]["col0"] if it["ii"] > 0 else None
            WT = WTs[g % 3]

            def f(p):
                last = None
                for (a_, b_) in segs(col0, QC):
                    last = p.matmul(Z[:, a_:b_], cb(C_NTRI_SUFF), SPt[:, a_:b_], start=False, stop=True, skip_group_check=True)
                if carry_in is not None:
                    for (a_, b_) in segs(pc0, QC):
                        last = p.matmul(Z[:, a_:b_], cb(C_NONES), carry_in[:, a_:b_], start=False, stop=True, skip_group_check=True)
                return last
            op(pe, f, [SPt.r, CB.r] + ([carry_in.r] if carry_in is not None else []), [Z.r])
            op(act, lambda a: a.activation(out=WT[:, col0:QC], in_=Z[:, col0:QC], func=AF.Exp), [Z.r], [WT.r])
            if kb * 128 >= q0:
                op(dve, lambda v: v.tensor_tensor(out=WT[:, col0:col0 + 128], in0=WT[:, col0:col0 + 128],
                                                  in1=cf(C_MSTRICT), op=ALU.mult), [WT.r, CF.r], [WT.r])
            it["WT"] = WT

        touched = {}

        def stage3(g):
            it = its[g]
            q0, j, hh, ii, nk, kb, WT, col0 = it["q0"], it["j"], it["hh"], it["ii"], it["nk"], it["kb"], it["WT"], it["col0"]
            b0 = 64 * hh
            tk = touched.setdefault((q0, j, hh), set())

            def f(p):
                last = None
                for (a_, b_) in segs(col0, QC):
                    bank = a_ // 512
                    first = bank not in tk
                    tk.add(bank)
                    last = p.matmul(PO_[:, a_:b_], VS[:, kb, 128 * j:128 * j + 128], WT[:, a_:b_],
                                    start=first, stop=(ii == nk - 1), skip_group_check=True)
                return last
            op(pe, f, [VS.r, WT.r], [PO_.r])
            if ii == nk - 1:
                op(act, lambda a: a.activation(out=OS[b0:b0 + 64, :], in_=PO_[b0:b0 + 64, :], func=AF.Identity), [PO_.r], [OS.r])
                if hh == 1:
                    op(act, lambda a: a.activation(out=SQn[:, :], in_=OS[:, :], func=AF.Square), [OS.r], [SQn.r])

                    def fn_(p):
                        last = None
                        for (a_, b_) in segs(0, QC):
                            last = p.matmul(SSn[:, a_:b_], cb(C_BD64), SQn[:, a_:b_], start=True, stop=True)
                        return last
                    op(pe, fn_, [SQn.r, CB.r], [SSn.r])
                    rstd_from_psum(SSn, 128, 64, RSn, w=QC)
                    op(dve, lambda v: v.scalar_tensor_tensor(out=OB[:, j, q0:q0 + QC], in0=OS[:, :], scalar=GK[:, 1:2], in1=RSn[:, :],
                                                             op0=ALU.mult, op1=ALU.mult), [OS.r, GK.r, RSn.r], [OB.r])

        SSn = PO_
        stage1(0)
        stage1(1)
        for g in range(NI):
            stage2(g)
            if g + 2 < NI:
                stage1(g + 2)
            if g >= 1:
                stage3(g - 1)
        stage3(NI - 1)
        fw.free(QT, QTH, KT, VS, GK, *Zp, PO_, *Es, *SPs, *WTs, *LSB, OS, SQn, RSn)
        tap(f"ob{l}", OB, [128, 3, T], BF16)
        maybe_stop(f"sbaB_{l}")
        return OB

    def conv(l):
        sc0 = l * SC_PER
        W = fw.tile("WC", [128, 8, 512], BF16)
        fw.dma_in(pool, W[:, :, :], win_d[l, :, 2320:2832].rearrange("(k p) c -> p k c", p=128), W.r)
        U = fw.tile("U", [128, 2, T + 32], BF16)
        DG = fw.tile("DG", [128, 2, 31, 128], BF16)
        G16 = fw.tile("G16", [128, 2], F32)
        op(dve, lambda v: v.tensor_scalar(out=G16[:, :], in0=SC[:, sc0 + SC_CLG:sc0 + SC_CLG + 2], scalar1=16.0, scalar2=None, op0=ALU.mult),
           [SC.r], [G16.r])
        for cc in range(2):
            op(dve, lambda v, cc=cc: v.memset(U[:, cc, 0:32], 0.0), [], [U.r])
            for jj in range(31):
                op(dve, lambda v, cc=cc, jj=jj: v.tensor_scalar(out=DG[:, cc, jj, :], in0=cf(C_IDENT),
                                                                 scalar1=SC[:, sc0 + SC_CW + cc * 31 + jj:sc0 + SC_CW + cc * 31 + jj + 1],
                                                                 scalar2=None, op0=ALU.mult), [CF.r, SC.r], [DG.r])
        Pa = [fw.ptile("Pca") for _ in range(2)]
        Pg = [fw.ptile("Pcg") for _ in range(2)]
        SGs = [fw.tile("SGc", [128, 512], F32) for _ in range(2)]
        cnt = 0
        for cc in range(2):
            for c4 in range(4):
                t0, t1 = c4 * 512, (c4 + 1) * 512
                PA = Pa[cnt % 2]
                PG = Pg[cnt % 2]
                SGt = SGs[cnt % 2]
                cnt += 1
                proj_fm(W, cc * 128, 128, t0, t1, PA)
                proj_fm(W, 256 + cc * 128, 128, t0, t1, PG)
                op(act, lambda a, PG=PG, SGt=SGt: a.activation(out=SGt[:, :], in_=PG[:, :], func=AF.Sigmoid), [PG.r], [SGt.r])
                op(dve, lambda v, PA=PA, SGt=SGt, cc=cc, t0=t0, t1=t1: v.tensor_tensor(out=U[:, cc, 32 + t0:32 + t1], in0=PA[:, :], in1=SGt[:, :],
                                                                                       op=ALU.mult), [PA.r, SGt.r], [U.r])
        fw.free(W, *Pg, *SGs)
        OC = fw.tile("OC", [128, 2, T], BF16)
        PCs = [fw.ptile("PC") for _ in range(4)]
        PM = Pa[0]
        PV = Pa[1]
        VB = fw.tile("VB", [128, 2, 512], F32)
        VBB = fw.tile("VBB", [128, 2, 512], BF16)
        XC = fw.tile("XC", [128, 2, 512], F32)
        SQc = fw.tile("SQc", [128, 2, 512], BF16)
        RSc = fw.tile("RSc", [128, 512], F32)
        def conv_mm(c4):
            t0 = c4 * 512
            for cc in range(2):
                PC = PCs[(c4 % 2) * 2 + cc]

                def f(p, PC=PC, cc=cc):
                    last = None
                    for jj in range(31):
                        last = p.matmul(PC[:, :], DG[:, cc, jj, :], U[:, cc, 2 + t0 + jj:2 + t0 + jj + 512], start=(jj == 0), stop=(jj == 30))
                    return last
                op(pe, f, [DG.r, U.r], [PC.r])
        conv_mm(0)
        for c4 in range(4):
            t0, t1 = c4 * 512, (c4 + 1) * 512
            if c4 + 1 < 4:
                conv_mm(c4 + 1)
            for cc in range(2):
                PC = PCs[(c4 % 2) * 2 + cc]
                op(act, lambda a, PC=PC, cc=cc: a.activation(out=VB[:, cc, :], in_=PC[:, :], func=AF.Identity,
                                                             bias=SC[:, sc0 + SC_CB + cc:sc0 + SC_CB + cc + 1]), [PC.r, SC.r], [VB.r])
                op(dve, lambda v, cc=cc: v.tensor_copy(out=VBB[:, cc, :], in_=VB[:, cc, :]), [VB.r], [VBB.r])
            op(pe, lambda p: (p.matmul(PM[:, :], cb(C_ONES), VBB[:, 0, :], start=True, stop=False),
                              p.matmul(PM[:, :], cb(C_ONES), VBB[:, 1, :], start=False, stop=True))[1], [VBB.r, CB.r], [PM.r])
            for cc in range(2):
                op(dve, lambda v, cc=cc: v.scalar_tensor_tensor(out=XC[:, cc, :], in0=PM[:, :], scalar=-1.0 / 256, in1=VB[:, cc, :],
                                                                 op0=ALU.mult, op1=ALU.add), [PM.r, VB.r], [XC.r])
                op(act, lambda a, cc=cc: a.activation(out=SQc[:, cc, :], in_=XC[:, cc, :], func=AF.Square), [XC.r], [SQc.r])
            op(pe, lambda p: (p.matmul(PV[:, :], cb(C_ONES), SQc[:, 0, :], start=True, stop=False),
                              p.matmul(PV[:, :], cb(C_ONES), SQc[:, 1, :], start=False, stop=True))[1], [SQc.r, CB.r], [PV.r])
            rstd_from_psum(PV, 128, 256, RSc)
            for cc in range(2):
                op(dve, lambda v, cc=cc: v.tensor_tensor(out=XC[:, cc, :], in0=XC[:, cc, :], in1=RSc[:, :], op=ALU.mult), [XC.r, RSc.r], [XC.r])
                op(act, lambda a, cc=cc: a.activation(out=OC[:, cc, t0:t1], in_=XC[:, cc, :], func=AF.Silu, scale=G16[:, cc:cc + 1],
                                                      bias=SC[:, sc0 + SC_CLB + cc:sc0 + SC_CLB + cc + 1]), [XC.r, G16.r, SC.r], [OC.r])
        fw.free(U, DG, G16, *PCs, *Pa, VB, VBB, XC, SQc, RSc)
        tap(f"oc{l}", OC, [128, 2, T], BF16)
        maybe_stop(f"conv_{l}")
        return OC

    def mixer(l, pre_normed=False):
        if not pre_normed:
            norm_modulate(l, 1)
        tap(f"hm{l}", H, [128, 8, T], BF16)
        OA = gla(l)
        OC = conv(l)
        OB = sba(l)
        WOA = fw.tile("WOA", [96, 4, D], BF16)
        WOB = fw.tile("WOB", [128, 3, D], BF16)
        WOC = fw.tile("WOC", [128, 2, D], BF16)
        fw.dma_in(pool, WOA[:, :, :], wout_d[l, 0:384, :].rearrange("(h p) c -> p h c", p=96), WOA.r)
        fw.dma_in(pool, WOB[:, :, :], wout_d[l, 384:768, :].rearrange("(h p) c -> p h c", p=128), WOB.r)
        fw.dma_in(pool, WOC[:, :, :], wout_d[l, 768:1024, :].rearrange("(h p) c -> p h c", p=128), WOC.r)
        G = DER[l][:, 4, :]
        PYs = [fw.ptile("PY") for _ in range(3)]
        cnt = 0
        nctx = norm_begin(l, 2, nss=2)
        for c4 in range(4):
            t0, t1 = c4 * 512, (c4 + 1) * 512
            for d in range(8):
                PY = PYs[cnt % 3]
                cnt += 1

                def f(p, PY=PY, d=d):
                    dc = slice(d * 128, (d + 1) * 128)
                    for h in range(4):
                        p.matmul(PY[:, :], WOA[0:96, h, dc], OA[0:96, h, t0:t1], start=(h == 0), stop=False)
                    for j in range(3):
                        p.matmul(PY[:, :], WOB[:, j, dc], OB[:, j, t0:t1], start=False, stop=False)
                    p.matmul(PY[:, :], WOC[:, 0, dc], OC[:, 0, t0:t1], start=False, stop=False)
                    return p.matmul(PY[:, :], WOC[:, 1, dc], OC[:, 1, t0:t1], start=False, stop=True)
                op(pe, f, [WOA.r, WOB.r, WOC.r, OA.r, OB.r, OC.r], [PY.r])
                op(dve, lambda v, PY=PY, d=d: v.scalar_tensor_tensor(out=X[:, d, t0:t1], in0=PY[:, :], scalar=G[:, d:d + 1],
                                                                      in1=X[:, d, t0:t1], op0=ALU.mult, op1=ALU.add),
                   [PY.r, DER[l].res[4]] + xr(t0, t1), xr(t0, t1))
            norm_chunk(nctx, c4)
        norm_end(nctx)
        fw.free(WOA, WOB, WOC, OA, OB, OC, *PYs)

    try:
        for l in layers:
            maybe_stop("ada")
            ffn(l, 0, pre_normed=(l != layers[0]), next_norm=(l, 1))
            tap(f"x{l}_a", X, [128, 8, T])
            maybe_stop(f"ffn1_{l}")
            mixer(l, pre_normed=True)
            tap(f"x{l}_b", X, [128, 8, T])
            maybe_stop(f"mix_{l}")
            ffn(l, 1, pre_normed=True, next_norm=((l + 1, 0) if l != layers[-1] else None))
            tap(f"x{l}_c", X, [128, 8, T])
    except _Stop:
        pass
    for r8 in range(8):
        fw.dma_out(sp, oT_d[:, r8 * 256:(r8 + 1) * 256].rearrange("(k p) t -> p k t", p=128),
                   X[:, :, r8 * 256:(r8 + 1) * 256], [X.res[r8]])
    fw.finish()
    nc._fw = fw
    return nc, tap_d


def _col(v, nchunk):
    return np.ascontiguousarray(v.reshape(nchunk, 128).T)


def prep_shared(inp):
    f = lambda a: np.ascontiguousarray(np.asarray(a, dtype=np.float32))
    sh = {}
    sh["w_ada"] = f(inp["w_ada"])
    sh["b_ada_col"] = np.concatenate([_col(f(inp["b_ada"])[l], 72) for l in range(NL)], axis=1)
    g = []
    for l in range(NL):
        for nm in ("norm_ffn1", "norm_mix", "norm_ffn2"):
            g.append(_col(f(inp[nm])[l], 8))
    sh["gains"] = np.ascontiguousarray(np.concatenate(g, axis=1))
    for nm in ("ffn1_w_in", "ffn1_w_out", "ffn2_w_in", "ffn2_w_out", "w_in", "w_out"):
        sh[nm] = f(inp[nm])
    w_in = sh["w_in"]
    wqk = np.zeros((NL, D, 512), np.float32)
    gup = np.zeros((NL, 16, 256), np.float32)
    gbg = np.zeros((NL, 1, 256), np.float32)
    up = f(inp["gla_w_gate_up"])
    bg = f(inp["gla_b_gate"])
    for h in range(4):
        wqk[:, :, 64 * h:64 * h + 48] = w_in[:, :, 48 * h:48 * h + 48]
        wqk[:, :, 256 + 64 * h:256 + 64 * h + 48] = w_in[:, :, 192 + 48 * h:192 + 48 * h + 48]
        gup[:, :, 64 * h:64 * h + 48] = up[:, :, 48 * h:48 * h + 48]
        gbg[:, 0, 64 * h:64 * h + 48] = bg[:, 48 * h:48 * h + 48]
    sh["w_in_qk"] = wqk
    sh["gla_up_pad"] = gup
    sh["gla_bg_pad"] = gbg
    sc = np.zeros((128, NL * SC_PER), np.float32)
    for l in range(NL):
        o = l * SC_PER
        sc[0:96, o + SC_GLA_ON] = f(inp["gla_out_norm"])[l]
        sc[:, o + SC_SBQ] = np.tile(f(inp["sb_q_norm"])[l], 2)
        sc[:, o + SC_SBK] = np.tile(f(inp["sb_k_norm"])[l], 2)
        sc[:, o + SC_SBO] = np.tile(f(inp["sb_out_norm"])[l], 2)
        sc[:, o + SC_CB:o + SC_CB + 2] = _col(f(inp["conv_b"])[l], 2)
        sc[:, o + SC_CLG:o + SC_CLG + 2] = _col(f(inp["conv_ln_g"])[l], 2)
        sc[:, o + SC_CLB:o + SC_CLB + 2] = _col(f(inp["conv_ln_b"])[l], 2)
        cw = f(inp["conv_w"])[l]
        for cc in range(2):
            sc[:, o + SC_CW + cc * 31:o + SC_CW + (cc + 1) * 31] = cw[:, cc * 128:(cc + 1) * 128].T
    sh["smallcols"] = sc
    sh["consts"] = np.ascontiguousarray(make_consts().reshape(128, NCONST * 128))
    return sh


def prep_core(inp, b):
    x = np.asarray(inp["x"], dtype=np.float32)
    c = np.asarray(inp["c"], dtype=np.float32)
    return {"xT": np.ascontiguousarray(x[b].T), "ccol": _col(c[b], 8)}


_CACHE = {}


def kernel(**inputs):
    if "nc" not in _CACHE:
        _CACHE["nc"] = build()[0]
    nc = _CACHE["nc"]
    sh = prep_shared(inputs)
    B = np.asarray(inputs["x"]).shape[0]
    in_maps = []
    for b in range(B):
        m = dict(sh)
        m.update(prep_core(inputs, b))
        in_maps.append(m)
    res = run_bass_kernel_spmd(nc, in_maps, core_ids=list(range(B)))
    out = np.stack([np.ascontiguousarray(r["oT"].T) for r in res.results], axis=0)
    return out.astype(np.float32)
```

```python
import math
import os
import numpy as np
LVL = int(os.environ.get('LVL', '9'))
import concourse.bass as bass
import concourse.mybir as mybir
from concourse.bass_utils import run_bass_kernel_spmd

F32 = mybir.dt.float32
BF16 = mybir.dt.bfloat16
AF = mybir.ActivationFunctionType
ALU = mybir.AluOpType

SB_BASE = 16512
SB_END = 229376

D = 1024
T = 2048
DFF = 2816
NL = 2
EPS = 1e-6
NFF = 22
FF_GROUPS = [(0, 4), (4, 8), (8, 12), (12, 16), (16, 19), (19, 22)]
IN_COLS = 2832


class Res:
    __slots__ = ("name", "w", "r", "dsem", "dcnt")

    def __init__(self, name, init=None):
        self.name = name
        self.w = None
        self.r = dict(init) if init else {}
        self.dsem = None
        self.dcnt = 0

    def state(self):
        d = dict(self.r)
        if self.w is not None:
            s, v = self.w
            if d.get(s, 0) < v:
                d[s] = v
        return d


class Eng:
    def __init__(self, name, h, sem, is_pe=False):
        self.name = name
        self.h = h
        self.sem = sem
        self.n = 0
        self.seen = {}
        self.is_pe = is_pe
        self.nwaits = 0
        self.nins = 0


class Arena:
    def __init__(self, lo, hi, align):
        self.free = [(lo, hi)]
        self.align = align
        self.grave = []
        self.peak = 0
        self.lo = lo

    def alloc(self, size):
        size = (size + self.align - 1) // self.align * self.align
        for i, (a, b) in enumerate(self.free):
            if b - a >= size:
                self.free[i] = (a + size, b)
                if self.free[i][0] == self.free[i][1]:
                    del self.free[i]
                self.peak = max(self.peak, a + size - self.lo)
                deps = {}
                keep = []
                for (ga, gb, gd) in self.grave:
                    if ga < a + size and gb > a:
                        for s, v in gd.items():
                            if deps.get(s, 0) < v:
                                deps[s] = v
                    keep.append((ga, gb, gd))
                self.grave = keep
                return a, size, deps
        raise RuntimeError(f"arena OOM size={size} free={self.free}")

    def release(self, a, size, deps):
        self.grave.append((a, a + size, deps))
        self.free.append((a, a + size))
        self.free.sort()
        m = []
        for seg in self.free:
            if m and m[-1][1] == seg[0]:
                m[-1] = (m[-1][0], seg[1])
            else:
                m.append(seg)
        self.free = m


class Tile:
    def __init__(self, t, res, addr, size, arena):
        self.t = t
        self.res = res
        self.addr = addr
        self.size = size
        self.arena = arena

    def __getitem__(self, k):
        return self.t[k]

    @property
    def r(self):
        return self.res[0]


class FW:
    def __init__(self, nc):
        self.nc = nc
        self.pe = Eng("pe", nc.tensor, self.new_sem("s_pe"), is_pe=True)
        self.act = Eng("act", nc.scalar, self.new_sem("s_act"))
        self.dve = Eng("dve", nc.vector, self.new_sem("s_dve"))
        self.pool = Eng("pool", nc.gpsimd, self.new_sem("s_pool"))
        self.sp = Eng("sp", nc.sync, self.new_sem("s_sp"))
        self.engs = [self.pe, self.act, self.dve, self.pool, self.sp]
        self.sb = Arena(SB_BASE, SB_END, 64)
        self.ps = Arena(0, 4096, 512)
        self.pst = nc.alloc_psum_tensor("ps", [128, 4096], F32)
        self.uid = 0
        self.out_sems = []
        self.free_dsems = []

    def new_sem(self, name):
        return self.nc.alloc_semaphore(name)

    def tile(self, name, shape, dtype, nres=1):
        nbytes = int(np.prod(shape[1:])) * (2 if dtype == BF16 else 4)
        a, size, deps = self.sb.alloc(nbytes)
        self.uid += 1
        t = self.nc.alloc_sbuf_tensor_at(f"{name}_{self.uid}", list(shape), dtype, offset=a)
        res = [Res(f"{name}{i}", deps) for i in range(nres)]
        return Tile(t, res, a, size, self.sb)

    def ptile(self, name, ncols=512, nres=1):
        a, size, deps = self.ps.alloc(ncols)
        res = [Res(f"{name}{i}", deps) for i in range(nres)]
        return Tile(self.pst[:, a:a + size], res, a, size, self.ps)

    def free(self, *tls):
        for tl in tls:
            deps = {}
            for r in tl.res:
                for s, v in r.state().items():
                    if deps.get(s, 0) < v:
                        deps[s] = v
                if r.dsem is not None:
                    self.free_dsems.append((r.dsem, r.dcnt))
                    r.dsem = None
            tl.arena.release(tl.addr, tl.size, deps)

    def _waits(self, eng, reads, writes):
        need = {}

        def add(s, v):
            if need.get(s, 0) < v:
                need[s] = v

        for r in reads:
            if r.w is not None:
                add(*r.w)
        for w in writes:
            if w.w is not None:
                add(*w.w)
            for s, v in w.r.items():
                if s is eng.sem:
                    continue
                add(s, v)
        for s, v in need.items():
            if eng.is_pe and s is eng.sem:
                continue
            if eng.seen.get(s, 0) >= v:
                continue
            eng.h.wait_ge(s, v)
            eng.nwaits += 1
            eng.seen[s] = v

    def op(self, eng, fn, reads=(), writes=()):
        self._waits(eng, reads, writes)
        ins = fn(eng.h)
        eng.n += 1
        ins.then_inc(eng.sem, 1)
        for r in reads:
            if r.r.get(eng.sem, 0) < eng.n:
                r.r[eng.sem] = eng.n
        for w in writes:
            w.w = (eng.sem, eng.n)
            w.r = {}

    def _get_dsem(self, r):
        if r.dsem is None:
            if self.free_dsems:
                r.dsem, r.dcnt = self.free_dsems.pop()
            else:
                self.uid += 1
                r.dsem = self.new_sem(f"d_{self.uid}")
                r.dcnt = 0
        return r.dsem

    def dma_in(self, q, out_ap, in_ap, wres):
        self._waits(q, (), (wres,))
        sem = self._get_dsem(wres)
        q.h.dma_start(out=out_ap, in_=in_ap).then_inc(sem, 16)
        wres.dcnt += 16
        wres.w = (sem, wres.dcnt)
        wres.r = {}

    def dma_out(self, q, out_ap, in_ap, rres):
        self._waits(q, rres, ())
        self.uid += 1
        sem = self.new_sem(f"o_{self.uid}")
        q.h.dma_start(out=out_ap, in_=in_ap).then_inc(sem, 16)
        self.out_sems.append(sem)
        for r in rres:
            r.r[sem] = 16

    def finish(self):
        for s in self.out_sems:
            self.sp.h.wait_ge(s, 16)
        for e in self.engs:
            if e is self.sp or e.n == 0:
                continue
            self.sp.h.wait_ge(e.sem, e.n)


C_ONES, C_BD64, C_TRI_INCL, C_TRI_GT, C_TRI_SUFF, C_MSTRICT, C_IDENT, C_NTRI_SUFF, C_NONES = range(9)
NCONST = 9
SC_GLA_ON, SC_SBQ, SC_SBK, SC_SBO, SC_CB, SC_CLG, SC_CLB, SC_CW = 0, 1, 2, 3, 4, 6, 8, 10
SC_PER = 72


def make_consts():
    p = np.arange(128)
    c = np.zeros((128, NCONST, 128), np.float32)
    c[:, C_ONES] = 1.0
    c[:, C_BD64] = (p[:, None] // 64 == p[None, :] // 64)
    same = (p[:, None] // 64 == p[None, :] // 64)
    c[:, C_TRI_INCL] = same & (p[:, None] <= p[None, :])
    c[:, C_TRI_GT] = same & (p[:, None] > p[None, :])
    c[:, C_TRI_SUFF] = (p[:, None] >= p[None, :])
    c[:, C_MSTRICT] = (p[:, None] < p[None, :])
    c[:, C_IDENT] = np.eye(128)
    c[:, C_NTRI_SUFF] = -c[:, C_TRI_SUFF]
    c[:, C_NONES] = -1.0
    return c


class _Stop(Exception):
    pass


def build(layers=(0, 1), taps=(), stop=None):
    nc = bass.Bass("TRN2", target_bir_lowering=False)

    def din(name, shape):
        return nc.dram_tensor(name, list(shape), F32, kind="ExternalInput").ap()

    xT_d = din("xT", [D, T])
    ccol_d = din("ccol", [128, 8])
    wada_d = din("w_ada", [NL, D, 9 * D])
    bada_d = din("b_ada_col", [128, NL * 72])
    gains_d = din("gains", [128, NL * 24])
    f1wi_d = din("ffn1_w_in", [NL, D, 2 * DFF])
    f1wo_d = din("ffn1_w_out", [NL, DFF, D])
    f2wi_d = din("ffn2_w_in", [NL, D, 2 * DFF])
    f2wo_d = din("ffn2_w_out", [NL, DFF, D])
    win_d = din("w_in", [NL, D, IN_COLS])
    wqk_d = din("w_in_qk", [NL, D, 512])
    wout_d = din("w_out", [NL, D, D])
    gup_d = din("gla_up_pad", [NL, 16, 256])
    gbg_d = din("gla_bg_pad", [NL, 1, 256])
    sc_d = din("smallcols", [128, NL * SC_PER])
    consts_d = din("consts", [128, NCONST * 128])
    oT_d = nc.dram_tensor("oT", [D, T], F32, kind="ExternalOutput").ap()
    tap_d = {}

    fw = FW(nc)
    pe, act, dve, pool, sp = fw.pe, fw.act, fw.dve, fw.pool, fw.sp
    op = fw.op

    def memrep(label):
        if os.environ.get('DEBUGMEM'):
            used = (SB_END - SB_BASE) - sum(b - a for a, b in fw.sb.free)
            print(f'[mem] {label}: used {used/1024:.1f} KB peak {fw.sb.peak/1024:.1f}')

    def maybe_stop(label):
        memrep(label)
        if stop == label:
            raise _Stop()

    def tap(name, tl, shape, dtype=F32):
        if name not in taps:
            return
        d = nc.dram_tensor("tap_" + name, list(shape), dtype, kind="ExternalOutput").ap()
        tap_d[name] = d
        src = tl.t
        if len(shape) == 2:
            src = src[:, :]
        elif len(shape) == 3:
            src = src[:, :, :]
        fw.dma_out(sp, d, src, tl.res)

    CF = fw.tile("CF", [128, NCONST, 128], F32)
    CB = fw.tile("CB", [128, NCONST, 128], BF16)
    SC = fw.tile("SC", [128, NL * SC_PER], F32)
    GN = fw.tile("GN", [128, NL * 24], F32)
    BA = fw.tile("BA", [128, NL * 72], F32)
    CC = fw.tile("CC", [128, 8], F32)
    fw.dma_in(sp, CF.t.rearrange("p a b -> p (a b)"), consts_d, CF.r)
    fw.dma_in(pool, CB.t.rearrange("p a b -> p (a b)"), consts_d, CB.r)
    fw.dma_in(sp, SC[:, :], sc_d, SC.r)
    fw.dma_in(sp, GN[:, :], gains_d, GN.r)
    fw.dma_in(sp, BA[:, :], bada_d, BA.r)
    fw.dma_in(sp, CC[:, :], ccol_d, CC.r)

    def cb(i):
        return CB[:, i, :]

    def cf(i):
        return CF[:, i, :]

    X = fw.tile("X", [128, 8, T], F32, nres=8)
    for r8 in range(8):
        fw.dma_in(sp, X[:, :, r8 * 256:(r8 + 1) * 256],
                  xT_d[:, r8 * 256:(r8 + 1) * 256].rearrange("(k p) t -> p k t", p=128), X.res[r8])

    def xr(t0, t1):
        return [X.res[i] for i in range(t0 // 256, (t1 + 255) // 256)]

    class _Holder:
        tl = None

        def __getitem__(self, k):
            return self.tl.t[k]

        @property
        def t(self):
            return self.tl.t

        @property
        def res(self):
            return self.tl.res

    H = _Holder()

    def h_alloc():
        H.tl = fw.tile("H", [128, 8, T], BF16, nres=8)

    def h_free():
        fw.free(H.tl)
        H.tl = None

    def hr(t0, t1):
        return [H.res[i] for i in range(t0 // 256, (t1 + 255) // 256)]

    CA = fw.tile("CA", [128, 8], BF16)
    op(act, lambda a: a.activation(out=CA[:, :], in_=CC[:, :], func=AF.Silu), [CC.r], [CA.r])
    MOD = {}
    DER = {}
    for l in layers:
        MOD[l] = fw.tile("MOD", [128, 72], F32, nres=9)
        DER[l] = fw.tile("DER", [128, 6, 8], F32, nres=6)
    ada = {"WAb": [fw.tile("WAda", [128, 8, 1024], BF16) for _ in range(2)], "MP": fw.ptile("MP"),
           "list": [(l, ch) for l in layers for ch in range(9)], "nf": 0, "nc": 0}

    def ada_prefetch():
        i = ada["nf"]
        if i >= len(ada["list"]):
            return
        ada["nf"] += 1
        l, ch = ada["list"][i]
        WA = ada["WAb"][i % 2]
        fw.dma_in(pool, WA[:, :, :], wada_d[l, :, ch * 1024:(ch + 1) * 1024].rearrange("(k p) c -> p k c", p=128), WA.r)

    def ada_compute():
        i = ada["nc"]
        if i >= len(ada["list"]):
            return False
        ada["nc"] += 1
        l, ch = ada["list"][i]
        WA = ada["WAb"][i % 2]
        MP = ada["MP"]
        pc = (i % 8) * 8

        def f(pe_):
            last = None
            for jj in range(8):
                for k in range(8):
                    last = pe_.matmul(MP[:, pc + jj:pc + jj + 1], WA[:, k, jj * 128:(jj + 1) * 128],
                                      CA[:, k:k + 1], start=(k == 0), stop=(k == 7))
            return last
        op(pe, f, [WA.r, CA.r], [MP.r])
        op(dve, lambda v: v.tensor_tensor(out=MOD[l][:, ch * 8:(ch + 1) * 8], in0=MP[:, pc:pc + 8],
                                          in1=BA[:, l * 72 + ch * 8:l * 72 + (ch + 1) * 8], op=ALU.add),
           [MP.r, BA.r], [MOD[l].res[ch]])
        n = ch // 3
        if ch % 3 == 1:
            op(dve, lambda v: v.tensor_scalar(out=DER[l][:, n, :], in0=MOD[l][:, ch * 8:(ch + 1) * 8],
                                              scalar1=1.0, scalar2=float(math.sqrt(D)), op0=ALU.add, op1=ALU.mult),
               [MOD[l].res[ch]], [DER[l].res[n]])
            op(dve, lambda v: v.tensor_tensor(out=DER[l][:, n, :], in0=DER[l][:, n, :],
                                              in1=GN[:, (l * 3 + n) * 8:(l * 3 + n + 1) * 8], op=ALU.mult),
               [DER[l].res[n], GN.r], [DER[l].res[n]])
        if ch % 3 == 2:
            op(dve, lambda v: v.tensor_scalar(out=DER[l][:, 3 + n, :], in0=MOD[l][:, ch * 8:(ch + 1) * 8],
                                              scalar1=(1.0 if n == 1 else 0.5), scalar2=None, op0=ALU.mult),
               [MOD[l].res[ch]], [DER[l].res[3 + n]])
        if ada["nc"] == len(ada["list"]):
            fw.free(ada["MP"], *ada["WAb"])
        return True

    def ada_step():
        if ada_compute():
            ada_prefetch()

    ada_prefetch()
    ada_prefetch()
    for _ in range(3):
        ada_step()

    def rstd_from_psum(SS, np_, nelem, out_tile, w=512):
        LNT = fw.tile("LNT", [128, w], F32)
        op(act, lambda a: a.activation(out=LNT[0:np_, 0:w], in_=SS[0:np_, 0:w], func=AF.Ln, bias=float(nelem * EPS)),
           [SS.r], [LNT.r])
        op(act, lambda a: a.activation(out=out_tile[0:np_, 0:w], in_=LNT[0:np_, 0:w], func=AF.Exp, scale=-0.5),
           [LNT.r], [out_tile.r])
        fw.free(LNT)

    def norm_begin(l, n, nss=2):
        if H.tl is None:
            h_alloc()
        return dict(l=l, n=n, cnt=0,
                    SQs=[fw.tile("SQ", [128, 512], BF16) for _ in range(4)],
                    TMs=[fw.tile("TM", [128, 512], F32) for _ in range(3)],
                    RSs=[fw.tile("RS", [128, 512], F32) for _ in range(2)],
                    SSs=[fw.ptile("SS") for _ in range(nss)])

    def norm_chunk(ctx, c4):
        l, n = ctx["l"], ctx["n"]
        A = DER[l][:, n, :]
        B = MOD[l][:, (3 * n) * 8:(3 * n + 1) * 8]
        t0, t1 = c4 * 512, (c4 + 1) * 512
        SS = ctx["SSs"][c4 % len(ctx["SSs"])]
        RS = ctx["RSs"][c4 % 2]
        for k in range(8):
            SQ = ctx["SQs"][ctx["cnt"] % 4]
            ctx["cnt"] += 1
            if k % 2 == 0:
                op(act, lambda a, SQ=SQ, k=k: a.activation(out=SQ[:, :], in_=X[:, k, t0:t1], func=AF.Square),
                   xr(t0, t1), [SQ.r])
            else:
                op(dve, lambda v, SQ=SQ, k=k: v.tensor_tensor(out=SQ[:, :], in0=X[:, k, t0:t1], in1=X[:, k, t0:t1], op=ALU.mult),
                   xr(t0, t1), [SQ.r])
            op(pe, lambda p, SQ=SQ, k=k, SS=SS: p.matmul(SS[:, :], cb(C_ONES), SQ[:, :], start=(k == 0), stop=(k == 7)),
               [SQ.r, CB.r], [SS.r])
        rstd_from_psum(SS, 128, D, RS)
        for k in range(8):
            TM = ctx["TMs"][ctx["cnt"] % 3]
            ctx["cnt"] += 1
            op(dve, lambda v, TM=TM, k=k, RS=RS: v.tensor_tensor(out=TM[:, :], in0=X[:, k, t0:t1], in1=RS[:, :], op=ALU.mult),
               xr(t0, t1) + [RS.r], [TM.r])
            op(act, lambda a, TM=TM, k=k: a.activation(out=H[:, k, t0:t1], in_=TM[:, :], func=AF.Identity,
                                                       scale=A[:, k:k + 1], bias=B[:, k:k + 1]),
               [TM.r, DER[l].res[n], MOD[l].res[3 * n]], hr(t0, t1))

    def norm_end(ctx):
        fw.free(*ctx["SQs"], *ctx["TMs"], *ctx["RSs"], *ctx["SSs"])

    def norm_modulate(l, n):
        ctx = norm_begin(l, n)
        for c4 in range(4):
            norm_chunk(ctx, c4)
        norm_end(ctx)

    def ffn(l, which, pre_normed=False, next_norm=None):
        wi_d = f1wi_d if which == 0 else f2wi_d
        wo_d = f1wo_d if which == 0 else f2wo_d
        n = 0 if which == 0 else 2
        G = DER[l][:, 3 + n, :]
        if not pre_normed:
            norm_modulate(l, n)
        nctx = None
        tap(f"h{l}_{which}", H, [128, 8, T], BF16)
        WA = [fw.tile("FWa", [128, 8, 512], BF16) for _ in range(2)]
        WB = [fw.tile("FWb", [128, 8, 512], BF16) for _ in range(2)]
        WO = [fw.tile("FWo", [128, 4, 1024], BF16) for _ in range(2)]
        SAs = [fw.tile("SA", [128, 256], F32) for _ in range(3)]
        GTs = [fw.tile("GT", [128, 256], BF16) for _ in range(3)]
        ABs = [fw.ptile("AB") for _ in range(3)]
        Y = fw.ptile("Y", 2048)

        def load(gi):
            c0, c1 = FF_GROUPS[gi]
            nchk = c1 - c0
            s = gi % 2
            fw.dma_in(pool, WA[s][:, :, 0:nchk * 128],
                      wi_d[l, :, c0 * 128:c1 * 128].rearrange("(k p) c -> p k c", p=128), WA[s].r)
            fw.dma_in(pool, WB[s][:, :, 0:nchk * 128],
                      wi_d[l, :, DFF + c0 * 128:DFF + c1 * 128].rearrange("(k p) c -> p k c", p=128), WB[s].r)
            fw.dma_in(pool, WO[s][:, 0:nchk, :],
                      wo_d[l, c0 * 128:c1 * 128, :].rearrange("(f p) c -> p f c", p=128), WO[s].r)

        load(0)
        cnt = 0
        it_count = [0]
        for gi, (c0, c1) in enumerate(FF_GROUPS):
            s = gi % 2
            if gi + 1 < len(FF_GROUPS):
                load(gi + 1)
            nchk = c1 - c0
            for tt in range(8):
                t0, t1 = tt * 256, (tt + 1) * 256
                pend = []

                def emit_ab(fi, cidx):
                    AB = ABs[cidx % 3]

                    def f(p):
                        last = None
                        for k in range(8):
                            p.matmul(AB[:, 0:256], WA[s][:, k, fi * 128:(fi + 1) * 128], H[:, k, t0:t1],
                                     start=(k == 0), stop=(k == 7))
                        for k in range(8):
                            last = p.matmul(AB[:, 256:512], WB[s][:, k, fi * 128:(fi + 1) * 128], H[:, k, t0:t1],
                                            start=(k == 0), stop=(k == 7))
                        return last
                    op(pe, f, [WA[s].r, WB[s].r] + hr(t0, t1), [AB.r])
                    return AB

                def emit_rest(fi, cidx, AB):
                    SA = SAs[cidx % 3]
                    GT = GTs[cidx % 3]
                    op(act, lambda a: a.activation(out=SA[:, :], in_=AB[:, 0:256], func=AF.Silu), [AB.r], [SA.r])
                    op(dve, lambda v: v.tensor_tensor(out=GT[:, :], in0=AB[:, 256:512], in1=SA[:, :], op=ALU.mult),
                       [AB.r, SA.r], [GT.r])

                    def f(p):
                        last = None
                        for d in range(8):
                            last = p.matmul(Y[:, d * 256:(d + 1) * 256], WO[s][:, fi, d * 128:(d + 1) * 128], GT[:, :],
                                            start=(fi == 0 and d % 2 == 0), stop=(fi == nchk - 1), skip_group_check=True)
                        return last
                    op(pe, f, [WO[s].r, GT.r], [Y.r])

                prev = None
                for fi in range(nchk):
                    AB = emit_ab(fi, cnt)
                    if prev is not None:
                        emit_rest(*prev)
                    prev = (fi, cnt, AB)
                    cnt += 1
                emit_rest(*prev)
                for d in range(8):
                    op(dve, lambda v, d=d: v.scalar_tensor_tensor(out=X[:, d, t0:t1], in0=Y[:, d * 256:(d + 1) * 256],
                                                                   scalar=G[:, d:d + 1], in1=X[:, d, t0:t1],
                                                                   op0=ALU.mult, op1=ALU.add),
                       [Y.r, DER[l].res[3 + n]] + xr(t0, t1), xr(t0, t1))
                it_count[0] += 1
                if it_count[0] % 2 == 0:
                    ada_step()
                if next_norm is not None and gi == len(FF_GROUPS) - 1:
                    if nctx is None:
                        nctx = norm_begin(next_norm[0], next_norm[1], nss=1)
                    if tt % 2 == 1:
                        norm_chunk(nctx, tt // 2)
        fw.free(*WA, *WB, *WO, *SAs, *GTs, *ABs, Y)
        if nctx is not None:
            norm_end(nctx)
        else:
            h_free()

    def proj_fm(Wt, col0, ncol, t0, t1, P, prow=None):
        def f(p):
            last = None
            for k in range(8):
                last = p.matmul(P[0:ncol, 0:t1 - t0], Wt[:, k, col0:col0 + ncol], H[:, k, t0:t1],
                                start=(k == 0), stop=(k == 7))
            return last
        op(pe, f, [Wt.r] + hr(t0, t1), [P.r])

    def proj_tm(Wt, col0, ncol, tok0, P, pcol0=0):
        def f(p):
            last = None
            for k in range(8):
                last = p.matmul(P[:, pcol0:pcol0 + ncol], H[:, k, tok0:tok0 + 128], Wt[:, k, col0:col0 + ncol],
                                start=(k == 0), stop=(k == 7))
            return last
        op(pe, f, [Wt.r] + hr(tok0, tok0 + 128), [P.r])

    def gla(l):
        sc0 = l * SC_PER
        WR = fw.tile("WR", [128, 8, 16], BF16)
        WUP = fw.tile("WUP", [16, 256], BF16)
        BG = fw.tile("BG", [1, 256], BF16)
        fw.dma_in(pool, WR[:, :, :], win_d[l, :, 1152:1168].rearrange("(k p) c -> p k c", p=128), WR.r)
        fw.dma_in(pool, WUP[:, :], gup_d[l], WUP.r)
        fw.dma_in(pool, BG[:, :], gbg_d[l], BG.r)
        WQK = fw.tile("WQK", [128, 8, 512], BF16)
        fw.dma_in(pool, WQK[:, :, :], wqk_d[l].rearrange("(k p) c -> p k c", p=128), WQK.r)
        GRT = fw.tile("GRT", [16, T], BF16)
        LA = fw.tile("LA", [128, 16, 256], BF16)
        Pa = [fw.ptile("Pa") for _ in range(2)]
        for c4 in range(4):
            P = Pa[c4 % 2]
            proj_fm(WR, 0, 16, c4 * 512, (c4 + 1) * 512, P)
            op(dve, lambda v, P=P, c4=c4: v.tensor_copy(out=GRT[0:16, c4 * 512:(c4 + 1) * 512], in_=P[0:16, :]), [P.r], [GRT.r])
        EZ = [fw.tile("EZ", [128, 512], F32) for _ in range(2)]
        for t2 in range(8):
            P = Pa[t2 % 2]

            def f(p, P=P, t2=t2):
                last = None
                for i in range(2):
                    tt = 2 * t2 + i
                    p.matmul(P[:, i * 256:(i + 1) * 256], GRT[0:16, tt * 128:(tt + 1) * 128], WUP[0:16, :], start=True, stop=False)
                    last = p.matmul(P[:, i * 256:(i + 1) * 256], CB[0:1, C_ONES, :], BG[0:1, :], start=False, stop=True)
                return last
            op(pe, f, [GRT.r, WUP.r, BG.r, CB.r], [P.r])
            E = EZ[t2 % 2]
            op(act, lambda a, P=P, E=E: a.activation(out=E[:, :], in_=P[:, :], func=AF.Exp, scale=-1.0), [P.r], [E.r])
            op(act, lambda a, E=E, t2=t2: a.activation(out=LA[:, 2 * t2:2 * t2 + 2, :].rearrange("p a b -> p (a b)"), in_=E[:, :],
                                                       func=AF.Ln, bias=1.0), [E.r], [LA.r])
        fw.free(WR, WUP, BG, GRT, *EZ)
        tap(f"la{l}", LA, [128, 16, 256], BF16)
        maybe_stop(f"glaA_{l}")
        QT = fw.tile("GQT", [128, 2, T], BF16)
        KT = fw.tile("GKT", [128, 2, T], BF16)
        DL = fw.tile("DL", [128, 2, 32], F32)
        KDL = fw.tile("KDL", [128, 16, 256], BF16)
        KDH = fw.tile("KDH", [128, 16, 256], BF16)
        Pb = [fw.ptile("Pb") for _ in range(2)]
        Pq = [fw.ptile("Pq") for _ in range(2)]
        EB = [fw.tile("EB", [128, 512], F32) for _ in range(2)]
        cnt = 0
        lnscale = float(math.log(48 ** -0.5))
        for c4 in range(4):
            t0, t1 = c4 * 512, (c4 + 1) * 512
            for j in range(2):
                PB = Pb[cnt % 2]

                def f(p, PB=PB, j=j, c4=c4):
                    last = None
                    for i in range(4):
                        tt = 4 * c4 + i
                        last = p.matmul(PB[:, i * 128:(i + 1) * 128], LA[:, tt, 128 * j:128 * j + 128], cb(C_TRI_INCL),
                                        start=True, stop=True)
                    return last
                op(pe, f, [LA.r, CB.r], [PB.r])
                op(act, lambda a, PB=PB, j=j, c4=c4: a.activation(out=DL[:, j, 8 * c4:8 * c4 + 8], in_=PB[:, 63::64],
                                                                   func=AF.Exp, scale=-1.0 / 16), [PB.r], [DL.r])
                for qk in range(2):
                    E = EB[cnt % 2]
                    PQ = Pq[cnt % 2]
                    cnt += 1
                    if qk == 0:
                        op(act, lambda a, PB=PB, E=E: a.activation(out=E[:, :], in_=PB[:, :], func=AF.Exp, scale=-1.0 / 16,
                                                                   bias=lnscale), [PB.r], [E.r])
                    else:
                        op(act, lambda a, PB=PB, E=E: a.activation(out=E[:, :], in_=PB[:, :], func=AF.Exp, scale=1.0 / 16),
                           [PB.r], [E.r])
                    proj_fm(WQK, qk * 256 + j * 128, 128, t0, t1, PQ)
                    dst = QT if qk == 0 else KT
                    op(dve, lambda v, PQ=PQ, E=E, dst=dst, j=j: v.tensor_tensor(out=dst[:, j, t0:t1], in0=PQ[:, :], in1=E[:, :],
                                                                                 op=ALU.mult), [PQ.r, E.r], [dst.r])
        for t2 in range(8):
            PK = Pq[t2 % 2]
            PB = Pb[t2 % 2]
            for i in range(2):
                proj_tm(WQK, 256, 256, (2 * t2 + i) * 128, PK, pcol0=i * 256)

            def f(p, PB=PB, t2=t2):
                last = None
                for i in range(2):
                    last = p.matmul(PB[:, i * 256:(i + 1) * 256], cb(C_TRI_GT), LA[:, 2 * t2 + i, :], start=True, stop=True)
                return last
            op(pe, f, [LA.r, CB.r], [PB.r])
            E = EB[t2 % 2]
            op(act, lambda a, PB=PB, E=E: a.activation(out=E[:, :], in_=PB[:, :], func=AF.Exp, scale=-1.0 / 16), [PB.r], [E.r])
            for KDx, mc in ((KDL, 0), (KDH, 127)):
                op(dve, lambda v, PK=PK, E=E, t2=t2, KDx=KDx, mc=mc: v.scalar_tensor_tensor(
                    out=KDx[:, 2 * t2:2 * t2 + 2, :].rearrange("p a b -> p (a b)"), in0=PK[:, :],
                    scalar=CF[:, C_BD64, mc:mc + 1], in1=E[:, :], op0=ALU.mult, op1=ALU.mult), [PK.r, E.r, CF.r], [KDx.r])
        maybe_stop(f"glaB_{l}")
        fw.free(WQK, LA, *EB, *Pb)
        WV = fw.tile("WV", [128, 8, 384], BF16)
        fw.dma_in(pool, WV[:, :, :], win_d[l, :, 384:768].rearrange("(k p) c -> p k c", p=128), WV.r)
        WG = fw.tile("WG", [128, 8, 384], BF16)
        fw.dma_in(pool, WG[:, :, :], win_d[l, :, 768:1152].rearrange("(k p) c -> p k c", p=128), WG.r)
        VG = fw.tile("VG", [128, 16, 384], BF16)
        for tt in range(16):
            P = Pq[tt % 2]
            proj_tm(WV, 0, 384, tt * 128, P)
            op(act, lambda a, P=P, tt=tt: a.activation(out=VG[:, tt, :], in_=P[:, 0:384], func=AF.Identity), [P.r], [VG.r])
        SG = fw.tile("SG", [96, 4, T], BF16)
        cnt = 0
        for h in range(4):
            for c4 in range(4):
                P = Pq[cnt % 2]
                cnt += 1
                proj_fm(WG, 96 * h, 96, c4 * 512, (c4 + 1) * 512, P)
                op(act, lambda a, P=P, h=h, c4=c4: a.activation(out=SG[0:96, h, c4 * 512:(c4 + 1) * 512], in_=P[0:96, :], func=AF.Silu),
                   [P.r], [SG.r])
        fw.free(WV, WG, *Pq, *Pa)
        tap(f"gqt{l}", QT, [128, 2, T], BF16)
        tap(f"gkt{l}", KT, [128, 2, T], BF16)
        tap(f"dl{l}", DL, [128, 2, 32])
        maybe_stop(f"glaC_{l}")
        GC = fw.tile("GC", [128, 1], F32)
        op(dve, lambda v: v.tensor_scalar(out=GC[0:96, :], in0=SC[0:96, sc0 + SC_GLA_ON:sc0 + SC_GLA_ON + 1],
                                          scalar1=float(math.sqrt(96)), scalar2=None, op0=ALU.mult), [SC.r], [GC.r])
        OA = fw.tile("OA", [96, 4, T], BF16)
        S = [fw.tile("S", [128, 192], F32) for _ in range(2)]
        SBF = [[fw.tile("SBF", [128, 192], BF16) for _ in range(4)] for _ in range(2)]
        for j in range(2):
            op(dve, lambda v, j=j: v.memset(S[j][:, :], 0.0), [], [S[j].r])
        PU = fw.ptile("PU")
        PSs = [fw.ptile("PS") for _ in range(2)]
        PO = [fw.ptile("PO") for _ in range(4)]
        SSo = fw.ptile("SSo")
        PTs = [fw.tile("PT", [128, 128], BF16) for _ in range(4)]
        SQo = fw.tile("SQo", [128, 512], BF16)
        RSo = fw.tile("RSo", [128, 512], F32)
        T1 = fw.tile("T1", [128, 512], F32)
        cnt = 0
        for c4 in range(4):
            for i4 in range(4):
                tt = 4 * c4 + i4
                tk0 = tt * 128
                for j in range(2):
                    def fu(p, tt=tt, j=j):
                        last = None
                        for ci in range(2):
                            KDx = KDL if ci == 0 else KDH
                            last = p.matmul(PU[:, ci * 192:(ci + 1) * 192], KDx[:, tt, 128 * j:128 * j + 128],
                                            VG[:, tt, 192 * j:192 * j + 192], start=True, stop=True)
                        return last
                    op(pe, fu, [KDL.r, KDH.r, VG.r], [PU.r])
                    pts = []
                    for hh in range(2):
                        b0 = 64 * hh
                        PS_ = PSs[hh]
                        PT = PTs[cnt % 4]
                        cnt += 1
                        op(pe, lambda p, PS_=PS_, b0=b0, j=j: p.matmul(PS_[:, 0:128], KT[b0:b0 + 48, j, tk0:tk0 + 128],
                                                                        QT[b0:b0 + 48, j, tk0:tk0 + 128], start=True, stop=True),
                           [KT.r, QT.r], [PS_.r])
                        op(dve, lambda v, PS_=PS_, PT=PT: v.tensor_tensor(out=PT[:, :], in0=PS_[:, 0:128], in1=cf(C_TRI_INCL), op=ALU.mult),
                           [PS_.r, CF.r], [PT.r])
                        pts.append(PT)
                    sbf = []
                    for ci in range(2):
                        c = 2 * tt + ci
                        sb_ = SBF[j][c % 4]
                        sbf.append(sb_)
                        op(dve, lambda v, sb_=sb_, j=j: v.tensor_copy(out=sb_[:, :], in_=S[j][:, :]), [S[j].r], [sb_.r])
                        op(dve, lambda v, j=j, c=c, ci=ci: v.scalar_tensor_tensor(out=S[j][:, :], in0=S[j][:, :], scalar=DL[:, j, c:c + 1],
                                                                                   in1=PU[:, ci * 192:(ci + 1) * 192],
                                                                                   op0=ALU.mult, op1=ALU.add),
                           [S[j].r, DL.r, PU.r], [S[j].r])
                    for hh in range(2):
                        h = 2 * j + hh
                        b0 = 64 * hh
                        PT = pts[hh]

                        def fo(p, PT=PT, h=h, hh=hh, b0=b0, j=j, i4=i4, sbf=sbf, tt=tt):
                            o = PO[h]
                            p.matmul(o[0:96, i4 * 128:(i4 + 1) * 128], VG[:, tt, 96 * h:96 * h + 96], PT[:, :], start=True, stop=False)
                            last = None
                            for ci in range(2):
                                last = p.matmul(o[0:96, i4 * 128 + 64 * ci:i4 * 128 + 64 * ci + 64],
                                                sbf[ci][b0:b0 + 48, 96 * hh:96 * hh + 96],
                                                QT[b0:b0 + 48, j, tk0 + 64 * ci:tk0 + 64 * ci + 64], start=False, stop=True)
                            return last
                        op(pe, fo, [VG.r, PT.r, sbf[0].r, sbf[1].r, QT.r], [PO[h].r])
            t0, t1 = c4 * 512, (c4 + 1) * 512
            for h in range(4):
                if LVL < 4:
                    break
                op(act, lambda a, h=h: a.activation(out=SQo[0:96, :], in_=PO[h][0:96, :], func=AF.Square), [PO[h].r], [SQo.r])
                op(pe, lambda p: p.matmul(SSo[0:96, :], CB[0:96, C_ONES, 0:96], SQo[0:96, :], start=True, stop=True), [SQo.r, CB.r], [SSo.r])
                rstd_from_psum(SSo, 96, 96, RSo)
                op(dve, lambda v, h=h: v.tensor_tensor(out=T1[0:96, :], in0=PO[h][0:96, :], in1=RSo[0:96, :], op=ALU.mult),
                   [PO[h].r, RSo.r], [T1.r])
                op(dve, lambda v, h=h: v.scalar_tensor_tensor(out=OA[0:96, h, t0:t1], in0=T1[0:96, :], scalar=GC[0:96, 0:1],
                                                               in1=SG[0:96, h, t0:t1], op0=ALU.mult, op1=ALU.mult),
                   [T1.r, GC.r, SG.r], [OA.r])
        fw.free(QT, KT, DL, KDL, KDH, VG, SG, GC, *S, *SBF[0], *SBF[1], PU, *PSs, *PO, SSo, *PTs, SQo, RSo, T1)
        tap(f"oa{l}", OA, [96, 4, T], BF16)
        maybe_stop(f"glaD_{l}")
        return OA

    def sba(l):
        sc0 = l * SC_PER
        Wqk = [None, None, None]

        def wload(i):
            Wt = fw.tile("WSB", [128, 8, 384], BF16)
            fw.dma_in(pool, Wt[:, :, :], win_d[l, :, 1168 + 384 * i:1168 + 384 * (i + 1)].rearrange("(k p) c -> p k c", p=128), Wt.r)
            Wqk[i] = Wt
        wload(0)
        wload(1)
        QT = fw.tile("SQT", [128, 3, T], BF16)
        QTH = fw.tile("SQTH", [128, 3, T], BF16)
        KT = fw.tile("SKT", [128, 3, T], BF16)
        VS = fw.tile("VS", [128, 16, 384], BF16)
        GK = fw.tile("GK", [128, 3], F32)
        op(dve, lambda v: v.tensor_tensor(out=GK[:, 2:3], in0=SC[:, sc0 + SC_SBQ:sc0 + SC_SBQ + 1], in1=CF[:, C_BD64, 127:128], op=ALU.mult),
           [SC.r, CF.r], [GK.r])
        op(dve, lambda v: v.tensor_scalar(out=GK[:, 0:1], in0=SC[:, sc0 + SC_SBK:sc0 + SC_SBK + 1], scalar1=8.0, scalar2=None, op0=ALU.mult),
           [SC.r], [GK.r])
        op(dve, lambda v: v.tensor_scalar(out=GK[:, 1:2], in0=SC[:, sc0 + SC_SBO:sc0 + SC_SBO + 1], scalar1=8.0, scalar2=None, op0=ALU.mult),
           [SC.r], [GK.r])
        Pp = [fw.ptile("Pp") for _ in range(2)]
        SSp = [fw.ptile("SSp") for _ in range(2)]
        SQp = [fw.tile("SQp", [128, 512], BF16) for _ in range(2)]
        RSp = [fw.tile("RSp", [128, 512], F32) for _ in range(2)]
        cnt = 0
        for qk in range(2):
            dst = QT if qk == 0 else KT
            gcol = SC[:, sc0 + SC_SBQ:sc0 + SC_SBQ + 1] if qk == 0 else GK[:, 0:1]
            for j in range(3):
                for c4 in range(4):
                    t0, t1 = c4 * 512, (c4 + 1) * 512
                    P = Pp[cnt % 2]
                    SS = SSp[cnt % 2]
                    SQ = SQp[cnt % 2]
                    RS = RSp[cnt % 2]
                    cnt += 1
                    proj_fm(Wqk[qk], j * 128, 128, t0, t1, P)
                    op(act, lambda a, P=P, SQ=SQ: a.activation(out=SQ[:, :], in_=P[:, :], func=AF.Square), [P.r], [SQ.r])
                    op(pe, lambda p, SS=SS, SQ=SQ: p.matmul(SS[:, :], cb(C_BD64), SQ[:, :], start=True, stop=True), [SQ.r, CB.r], [SS.r])
                    rstd_from_psum(SS, 128, 64, RS)
                    op(dve, lambda v, P=P, RS=RS, dst=dst, j=j, gcol=gcol, t0=t0, t1=t1: v.scalar_tensor_tensor(
                        out=dst[:, j, t0:t1], in0=P[:, :], scalar=gcol, in1=RS[:, :], op0=ALU.mult, op1=ALU.mult),
                       [P.r, RS.r, SC.r, GK.r], [dst.r])
                    if qk == 0:
                        op(dve, lambda v, P=P, RS=RS, j=j, t0=t0, t1=t1: v.scalar_tensor_tensor(
                            out=QTH[:, j, t0:t1], in0=P[:, :], scalar=GK[:, 2:3], in1=RS[:, :], op0=ALU.mult, op1=ALU.mult),
                           [P.r, RS.r, GK.r], [QTH.r])
        fw.free(Wqk[0], Wqk[1])
        wload(2)
        for tt in range(16):
            P = Pp[tt % 2]
            proj_tm(Wqk[2], 0, 384, tt * 128, P)
            op(act, lambda a, P=P, tt=tt: a.activation(out=VS[:, tt, :], in_=P[:, 0:384], func=AF.Identity), [P.r], [VS.r])
        fw.free(Wqk[2], *Pp, *SSp, *SQp, *RSp)
        h_free()
        tap(f"sqt{l}", QT, [128, 3, T], BF16)
        tap(f"skt{l}", KT, [128, 3, T], BF16)
        tap(f"vs{l}", VS, [128, 16, 384], BF16)
        maybe_stop(f"sbaA_{l}")
        OB = fw.tile("OB", [128, 3, T], BF16)
        QC = 1024

        def segs(c0, c1):
            out = []
            c = c0
            while c < c1:
                e = min(c1, (c // 512 + 1) * 512)
                out.append((c, e))
                c = e
            return out
        Zp = [fw.ptile("Zp", QC) for _ in range(3)]
        PO_ = fw.ptile("POs", QC)
        Es = [fw.tile("E", [128, QC], F32) for _ in range(2)]
        SPs = [fw.tile("SP", [128, QC], BF16) for _ in range(4)]
        WTs = [fw.tile("WT", [128, QC], BF16) for _ in range(3)]
        LSB = [fw.tile("LSB", [128, QC], BF16) for _ in range(4)]
        OS = fw.tile("OS", [128, QC], F32)
        SQn = fw.tile("SQn", [128, QC], BF16)
        RSn = fw.tile("RSn", [128, QC], F32)
        its = []
        for qc in range(T // QC):
            q0 = qc * QC
            for j in range(3):
                for hh in range(2):
                    kbs = list(range((q0 + QC) // 128 - 1, -1, -1))
                    for ii, kb in enumerate(kbs):
                        its.append(dict(q0=q0, j=j, hh=hh, ii=ii, nk=len(kbs), kb=kb, col0=max(0, kb * 128 - q0)))
        NI = len(its)

        def stage1(g):
            it = its[g]
            q0, j, hh, ii, nk, kb, col0 = it["q0"], it["j"], it["hh"], it["ii"], it["nk"], it["kb"], it["col0"]
            Z = Zp[g % 3]
            E = Es[g % 2]
            SPt = SPs[g % 4]

            def fz(p):
                last = None
                for (a_, b_) in segs(col0, QC):
                    if hh == 0:
                        last = p.matmul(Z[:, a_:b_], KT[0:64, j, kb * 128:(kb + 1) * 128], QT[0:64, j, q0 + a_:q0 + b_],
                                        start=True, stop=False, skip_group_check=True)
                    else:
                        last = p.matmul(Z[:, a_:b_], KT[:, j, kb * 128:(kb + 1) * 128], QTH[:, j, q0 + a_:q0 + b_],
                                        start=True, stop=False, skip_group_check=True)
                return last
            op(pe, fz, [KT.r, QT.r, QTH.r], [Z.r])
            op(act, lambda a: a.activation(out=E[:, col0:QC], in_=Z[:, col0:QC], func=AF.Exp), [Z.r], [E.r])
            op(act, lambda a: a.activation(out=SPt[:, col0:QC], in_=E[:, col0:QC], func=AF.Ln, bias=1.0), [E.r], [SPt.r])
            if kb * 128 >= q0:
                op(dve, lambda v: v.tensor_tensor(out=SPt[:, col0:col0 + 128], in0=SPt[:, col0:col0 + 128],
                                                  in1=cf(C_MSTRICT), op=ALU.mult), [SPt.r, CF.r], [SPt.r])
            carry = None
            if ii + 1 < nk:
                if ii == 0:
                    carry = SPt
                else:
                    prev = its[g - 1]["carry"]
                    pc0 = its[g - 1]["col0"]
                    carry = LSB[g % 4]
                    op(dve, lambda v: v.tensor_tensor(out=carry[:, pc0:QC], in0=prev[:, pc0:QC], in1=SPt[:, pc0:QC], op=ALU.add),
                       [prev.r, SPt.r], [carry.r])
                    if col0 < pc0:
                        op(dve, lambda v: v.tensor_copy(out=carry[:, col0:pc0], in_=SPt[:, col0:pc0]), [SPt.r], [carry.r])
            it["Z"], it["SP"], it["carry"] = Z, SPt, carry

        def stage2(g):
            it = its[g]
            q0, kb, col0, Z, SPt = it["q0"], it["kb"], it["col0"], it["Z"], it["SP"]
            carry_in = its[g - 1]["carry"] if it["ii"] > 0 else None
            pc0 = its[g - 1]["col0"] if it["ii"] > 0 else None
            WT = WTs[g % 3]

            def f(p):
                last = None
                for (a_, b_) in segs(col0, QC):
                    last = p.matmul(Z[:, a_:b_], cb(C_NTRI_SUFF), SPt[:, a_:b_], start=False, stop=True, skip_group_check=True)
                if carry_in is not None:
                    for (a_, b_) in segs(pc0, QC):
                        last = p.matmul(Z[:, a_:b_], cb(C_NONES), carry_in[:, a_:b_], start=False, stop=True, skip_group_check=True)
                return last
            op(pe, f, [SPt.r, CB.r] + ([carry_in.r] if carry_in is not None else []), [Z.r])
            op(act, lambda a: a.activation(out=WT[:, col0:QC], in_=Z[:, col0:QC], func=AF.Exp), [Z.r], [WT.r])
            if kb * 128 >= q0:
                op(dve, lambda v: v.tensor_tensor(out=WT[:, col0:col0 + 128], in0=WT[:, col0:col0 + 128],
                                                  in1=cf(C_MSTRICT), op=ALU.mult), [WT.r, CF.r], [WT.r])
            it["WT"] = WT

        touched = {}

        def stage3(g):
            it = its[g]
            q0, j, hh, ii, nk, kb, WT, col0 = it["q0"], it["j"], it["hh"], it["ii"], it["nk"], it["kb"], it["WT"], it["col0"]
            b0 = 64 * hh
            tk = touched.setdefault((q0, j, hh), set())

            def f(p):
                last = None
                for (a_, b_) in segs(col0, QC):
                    bank = a_ // 512
                    first = bank not in tk
                    tk.add(bank)
                    last = p.matmul(PO_[:, a_:b_], VS[:, kb, 128 * j:128 * j + 128], WT[:, a_:b_],
                                    start=first, stop=(ii == nk - 1), skip_group_check=True)
                return last
            op(pe, f, [VS.r, WT.r], [PO_.r])
            if ii == nk - 1:
                op(act, lambda a: a.activation(out=OS[b0:b0 + 64, :], in_=PO_[b0:b0 + 64, :], func=AF.Identity), [PO_.r], [OS.r])
                if hh == 1:
                    op(act, lambda a: a.activation(out=SQn[:, :], in_=OS[:, :], func=AF.Square), [OS.r], [SQn.r])

                    def fn_(p):
                        last = None
                        for (a_, b_) in segs(0, QC):
                            last = p.matmul(SSn[:, a_:b_], cb(C_BD64), SQn[:, a_:b_], start=True, stop=True)
                        return last
                    op(pe, fn_, [SQn.r, CB.r], [SSn.r])
                    rstd_from_psum(SSn, 128, 64, RSn, w=QC)
                    op(dve, lambda v: v.scalar_tensor_tensor(out=OB[:, j, q0:q0 + QC], in0=OS[:, :], scalar=GK[:, 1:2], in1=RSn[:, :],
                                                             op0=ALU.mult, op1=ALU.mult), [OS.r, GK.r, RSn.r], [OB.r])

        SSn = PO_
        stage1(0)
        stage1(1)
        for g in range(NI):
            stage2(g)
            if g + 2 < NI:
                stage1(g + 2)
            if g >= 1:
                stage3(g - 1)
        stage3(NI - 1)
        fw.free(QT, QTH, KT, VS, GK, *Zp, PO_, *Es, *SPs, *WTs, *LSB, OS, SQn, RSn)
        tap(f"ob{l}", OB, [128, 3, T], BF16)
        maybe_stop(f"sbaB_{l}")
        return OB

    def conv(l):
        sc0 = l * SC_PER
        W = fw.tile("WC", [128, 8, 512], BF16)
        fw.dma_in(pool, W[:, :, :], win_d[l, :, 2320:2832].rearrange("(k p) c -> p k c", p=128), W.r)
        U = fw.tile("U", [128, 2, T + 32], BF16)
        DG = fw.tile("DG", [128, 2, 31, 128], BF16)
        G16 = fw.tile("G16", [128, 2], F32)
        op(dve, lambda v: v.tensor_scalar(out=G16[:, :], in0=SC[:, sc0 + SC_CLG:sc0 + SC_CLG + 2], scalar1=16.0, scalar2=None, op0=ALU.mult),
           [SC.r], [G16.r])
        for cc in range(2):
            op(dve, lambda v, cc=cc: v.memset(U[:, cc, 0:32], 0.0), [], [U.r])
            for jj in range(31):
                op(dve, lambda v, cc=cc, jj=jj: v.tensor_scalar(out=DG[:, cc, jj, :], in0=cf(C_IDENT),
                                                                 scalar1=SC[:, sc0 + SC_CW + cc * 31 + jj:sc0 + SC_CW + cc * 31 + jj + 1],
                                                                 scalar2=None, op0=ALU.mult), [CF.r, SC.r], [DG.r])
        Pa = [fw.ptile("Pca") for _ in range(2)]
        Pg = [fw.ptile("Pcg") for _ in range(2)]
        SGs = [fw.tile("SGc", [128, 512], F32) for _ in range(2)]
        cnt = 0
        for cc in range(2):
            for c4 in range(4):
                t0, t1 = c4 * 512, (c4 + 1) * 512
                PA = Pa[cnt % 2]
                PG = Pg[cnt % 2]
                SGt = SGs[cnt % 2]
                cnt += 1
                proj_fm(W, cc * 128, 128, t0, t1, PA)
                proj_fm(W, 256 + cc * 128, 128, t0, t1, PG)
                op(act, lambda a, PG=PG, SGt=SGt: a.activation(out=SGt[:, :], in_=PG[:, :], func=AF.Sigmoid), [PG.r], [SGt.r])
                op(dve, lambda v, PA=PA, SGt=SGt, cc=cc, t0=t0, t1=t1: v.tensor_tensor(out=U[:, cc, 32 + t0:32 + t1], in0=PA[:, :], in1=SGt[:, :],
                                                                                       op=ALU.mult), [PA.r, SGt.r], [U.r])
        fw.free(W, *Pg, *SGs)
        OC = fw.tile("OC", [128, 2, T], BF16)
        PCs = [fw.ptile("PC") for _ in range(4)]
        PM = Pa[0]
        PV = Pa[1]
        VB = fw.tile("VB", [128, 2, 512], F32)
        VBB = fw.tile("VBB", [128, 2, 512], BF16)
        XC = fw.tile("XC", [128, 2, 512], F32)
        SQc = fw.tile("SQc", [128, 2, 512], BF16)
        RSc = fw.tile("RSc", [128, 512], F32)
        def conv_mm(c4):
            t0 = c4 * 512
            for cc in range(2):
                PC = PCs[(c4 % 2) * 2 + cc]

                def f(p, PC=PC, cc=cc):
                    last = None
                    for jj in range(31):
                        last = p.matmul(PC[:, :], DG[:, cc, jj, :], U[:, cc, 2 + t0 + jj:2 + t0 + jj + 512], start=(jj == 0), stop=(jj == 30))
                    return last
                op(pe, f, [DG.r, U.r], [PC.r])
        conv_mm(0)
        for c4 in range(4):
            t0, t1 = c4 * 512, (c4 + 1) * 512
            if c4 + 1 < 4:
                conv_mm(c4 + 1)
            for cc in range(2):
                PC = PCs[(c4 % 2) * 2 + cc]
                op(act, lambda a, PC=PC, cc=cc: a.activation(out=VB[:, cc, :], in_=PC[:, :], func=AF.Identity,
                                                             bias=SC[:, sc0 + SC_CB + cc:sc0 + SC_CB + cc + 1]), [PC.r, SC.r], [VB.r])
                op(dve, lambda v, cc=cc: v.tensor_copy(out=VBB[:, cc, :], in_=VB[:, cc, :]), [VB.r], [VBB.r])
            op(pe, lambda p: (p.matmul(PM[:, :], cb(C_ONES), VBB[:, 0, :], start=True, stop=False),
                              p.matmul(PM[:, :], cb(C_ONES), VBB[:, 1, :], start=False, stop=True))[1], [VBB.r, CB.r], [PM.r])
            for cc in range(2):
                op(dve, lambda v, cc=cc: v.scalar_tensor_tensor(out=XC[:, cc, :], in0=PM[:, :], scalar=-1.0 / 256, in1=VB[:, cc, :],
                                                                 op0=ALU.mult, op1=ALU.add), [PM.r, VB.r], [XC.r])
                op(act, lambda a, cc=cc: a.activation(out=SQc[:, cc, :], in_=XC[:, cc, :], func=AF.Square), [XC.r], [SQc.r])
            op(pe, lambda p: (p.matmul(PV[:, :], cb(C_ONES), SQc[:, 0, :], start=True, stop=False),
                              p.matmul(PV[:, :], cb(C_ONES), SQc[:, 1, :], start=False, stop=True))[1], [SQc.r, CB.r], [PV.r])
            rstd_from_psum(PV, 128, 256, RSc)
            for cc in range(2):
                op(dve, lambda v, cc=cc: v.tensor_tensor(out=XC[:, cc, :], in0=XC[:, cc, :], in1=RSc[:, :], op=ALU.mult), [XC.r, RSc.r], [XC.r])
                op(act, lambda a, cc=cc: a.activation(out=OC[:, cc, t0:t1], in_=XC[:, cc, :], func=AF.Silu, scale=G16[:, cc:cc + 1],
                                                      bias=SC[:, sc0 + SC_CLB + cc:sc0 + SC_CLB + cc + 1]), [XC.r, G16.r, SC.r], [OC.r])
        fw.free(U, DG, G16, *PCs, *Pa, VB, VBB, XC, SQc, RSc)
        tap(f"oc{l}", OC, [128, 2, T], BF16)
        maybe_stop(f"conv_{l}")
        return OC

    def mixer(l, pre_normed=False):
        if not pre_normed:
            norm_modulate(l, 1)
        tap(f"hm{l}", H, [128, 8, T], BF16)
        OA = gla(l)
        OC = conv(l)
        OB = sba(l)
        WOA = fw.tile("WOA", [96, 4, D], BF16)
        WOB = fw.tile("WOB", [128, 3, D], BF16)
        WOC = fw.tile("WOC", [128, 2, D], BF16)
        fw.dma_in(pool, WOA[:, :, :], wout_d[l, 0:384, :].rearrange("(h p) c -> p h c", p=96), WOA.r)
        fw.dma_in(pool, WOB[:, :, :], wout_d[l, 384:768, :].rearrange("(h p) c -> p h c", p=128), WOB.r)
        fw.dma_in(pool, WOC[:, :, :], wout_d[l, 768:1024, :].rearrange("(h p) c -> p h c", p=128), WOC.r)
        G = DER[l][:, 4, :]
        PYs = [fw.ptile("PY") for _ in range(3)]
        cnt = 0
        nctx = norm_begin(l, 2, nss=2)
        for c4 in range(4):
            t0, t1 = c4 * 512, (c4 + 1) * 512
            for d in range(8):
                PY = PYs[cnt % 3]
                cnt += 1

                def f(p, PY=PY, d=d):
                    dc = slice(d * 128, (d + 1) * 128)
                    for h in range(4):
                        p.matmul(PY[:, :], WOA[0:96, h, dc], OA[0:96, h, t0:t1], start=(h == 0), stop=False)
                    for j in range(3):
                        p.matmul(PY[:, :], WOB[:, j, dc], OB[:, j, t0:t1], start=False, stop=False)
                    p.matmul(PY[:, :], WOC[:, 0, dc], OC[:, 0, t0:t1], start=False, stop=False)
                    return p.matmul(PY[:, :], WOC[:, 1, dc], OC[:, 1, t0:t1], start=False, stop=True)
                op(pe, f, [WOA.r, WOB.r, WOC.r, OA.r, OB.r, OC.r], [PY.r])
                op(dve, lambda v, PY=PY, d=d: v.scalar_tensor_tensor(out=X[:, d, t0:t1], in0=PY[:, :], scalar=G[:, d:d + 1],
                                                                      in1=X[:, d, t0:t1], op0=ALU.mult, op1=ALU.add),
                   [PY.r, DER[l].res[4]] + xr(t0, t1), xr(t0, t1))
            norm_chunk(nctx, c4)
        norm_end(nctx)
        fw.free(WOA, WOB, WOC, OA, OB, OC, *PYs)

    try:
        for l in layers:
            maybe_stop("ada")
            ffn(l, 0, pre_normed=(l != layers[0]), next_norm=(l, 1))
            tap(f"x{l}_a", X, [128, 8, T])
            maybe_stop(f"ffn1_{l}")
            mixer(l, pre_normed=True)
            tap(f"x{l}_b", X, [128, 8, T])
            maybe_stop(f"mix_{l}")
            ffn(l, 1, pre_normed=True, next_norm=((l + 1, 0) if l != layers[-1] else None))
            tap(f"x{l}_c", X, [128, 8, T])
    except _Stop:
        pass
    for r8 in range(8):
        fw.dma_out(sp, oT_d[:, r8 * 256:(r8 + 1) * 256].rearrange("(k p) t -> p k t", p=128),
                   X[:, :, r8 * 256:(r8 + 1) * 256], [X.res[r8]])
    fw.finish()
    nc._fw = fw
    return nc, tap_d


def _col(v, nchunk):
    return np.ascontiguousarray(v.reshape(nchunk, 128).T)


def prep_shared(inp):
    f = lambda a: np.ascontiguousarray(np.asarray(a, dtype=np.float32))
    sh = {}
    sh["w_ada"] = f(inp["w_ada"])
    sh["b_ada_col"] = np.concatenate([_col(f(inp["b_ada"])[l], 72) for l in range(NL)], axis=1)
    g = []
    for l in range(NL):
        for nm in ("norm_ffn1", "norm_mix", "norm_ffn2"):
            g.append(_col(f(inp[nm])[l], 8))
    sh["gains"] = np.ascontiguousarray(np.concatenate(g, axis=1))
    for nm in ("ffn1_w_in", "ffn1_w_out", "ffn2_w_in", "ffn2_w_out", "w_in", "w_out"):
        sh[nm] = f(inp[nm])
    w_in = sh["w_in"]
    wqk = np.zeros((NL, D, 512), np.float32)
    gup = np.zeros((NL, 16, 256), np.float32)
    gbg = np.zeros((NL, 1, 256), np.float32)
    up = f(inp["gla_w_gate_up"])
    bg = f(inp["gla_b_gate"])
    for h in range(4):
        wqk[:, :, 64 * h:64 * h + 48] = w_in[:, :, 48 * h:48 * h + 48]
        wqk[:, :, 256 + 64 * h:256 + 64 * h + 48] = w_in[:, :, 192 + 48 * h:192 + 48 * h + 48]
        gup[:, :, 64 * h:64 * h + 48] = up[:, :, 48 * h:48 * h + 48]
        gbg[:, 0, 64 * h:64 * h + 48] = bg[:, 48 * h:48 * h + 48]
    sh["w_in_qk"] = wqk
    sh["gla_up_pad"] = gup
    sh["gla_bg_pad"] = gbg
    sc = np.zeros((128, NL * SC_PER), np.float32)
    for l in range(NL):
        o = l * SC_PER
        sc[0:96, o + SC_GLA_ON] = f(inp["gla_out_norm"])[l]
        sc[:, o + SC_SBQ] = np.tile(f(inp["sb_q_norm"])[l], 2)
        sc[:, o + SC_SBK] = np.tile(f(inp["sb_k_norm"])[l], 2)
        sc[:, o + SC_SBO] = np.tile(f(inp["sb_out_norm"])[l], 2)
        sc[:, o + SC_CB:o + SC_CB + 2] = _col(f(inp["conv_b"])[l], 2)
        sc[:, o + SC_CLG:o + SC_CLG + 2] = _col(f(inp["conv_ln_g"])[l], 2)
        sc[:, o + SC_CLB:o + SC_CLB + 2] = _col(f(inp["conv_ln_b"])[l], 2)
        cw = f(inp["conv_w"])[l]
        for cc in range(2):
            sc[:, o + SC_CW + cc * 31:o + SC_CW + (cc + 1) * 31] = cw[:, cc * 128:(cc + 1) * 128].T
    sh["smallcols"] = sc
    sh["consts"] = np.ascontiguousarray(make_consts().reshape(128, NCONST * 128))
    return sh


def prep_core(inp, b):
    x = np.asarray(inp["x"], dtype=np.float32)
    c = np.asarray(inp["c"], dtype=np.float32)
    return {"xT": np.ascontiguousarray(x[b].T), "ccol": _col(c[b], 8)}


_CACHE = {}


def kernel(**inputs):
    if "nc" not in _CACHE:
        _CACHE["nc"] = build()[0]
    nc = _CACHE["nc"]
    sh = prep_shared(inputs)
    B = np.asarray(inputs["x"]).shape[0]
    in_maps = []
    for b in range(B):
        m = dict(sh)
        m.update(prep_core(inputs, b))
        in_maps.append(m)
    res = run_bass_kernel_spmd(nc, in_maps, core_ids=list(range(B)))
    out = np.stack([np.ascontiguousarray(r["oT"].T) for r in res.results], axis=0)
    return out.astype(np.float32)
```
